# Optimizing a Trainium2 kernel written in Bass

```python
import math
import jax, jax.numpy as jnp
from jax import lax
import numpy as np

D_MODEL = 1024
BATCH = 8
SEQ = 4096
DEPTH = 1

CHUNK = 64
N_META = 16
N_PAD = CHUNK - N_META
EPS = 1e-6

SSD_EXPAND = 2
SSD_D_INNER = SSD_EXPAND * D_MODEL
SSD_HEAD_DIM = 64
SSD_N_HEADS = SSD_D_INNER // SSD_HEAD_DIM
SSD_N_GROUPS = 8
SSD_HEADS_PER_GROUP = SSD_N_HEADS // SSD_N_GROUPS
SSD_D_STATE = 128
SSD_CONV_W = 4
SSD_CONV_DIM = SSD_D_INNER + 2 * SSD_N_GROUPS * SSD_D_STATE

SC_WIDTH = D_MODEL
SC_CONV_W = 3

N_BRANCH = 2

PEER_HEADS = 8
PEER_N_KEYS = 128
PEER_N_EXPERTS = PEER_N_KEYS * PEER_N_KEYS
PEER_D_KEY = 256
PEER_HALF = PEER_D_KEY // 2
PEER_TOPK = 16
PEER_BLOCK = CHUNK

PROJ_SIZES = (
    SSD_D_INNER,
    SSD_D_INNER,
    SSD_N_GROUPS * SSD_D_STATE,
    SSD_N_GROUPS * SSD_D_STATE,
    SSD_N_HEADS,
    SC_WIDTH,
    SC_WIDTH,
    SC_WIDTH,
    N_BRANCH * D_MODEL,
)
PROJ_TOTAL = sum(PROJ_SIZES)

kernel_name = "hybrid_ssd_shortconv_peer_block"


def rmsnorm(x, g):
    xf = x.astype(jnp.float32)
    y = xf * lax.rsqrt(jnp.mean(xf * xf, axis=-1, keepdims=True) + EPS)
    return (y * g).astype(x.dtype)


def group_rmsnorm(y, g, groups):
    shp = y.shape
    yg = y.reshape(shp[:-1] + (groups, shp[-1] // groups)).astype(jnp.float32)
    yg = yg * lax.rsqrt(jnp.mean(yg * yg, axis=-1, keepdims=True) + EPS)
    return yg.reshape(shp) * g


def causal_dwconv(x, w):
    K = w.shape[-1]
    L = x.shape[1]
    xp = jnp.pad(x, ((0, 0), (K - 1, 0), (0, 0)))
    y = xp[:, 0:L, :] * w[:, 0]
    for k in range(1, K):
        y = y + xp[:, k:k + L, :] * w[:, k]
    return y


def ssd_chunked_scan(xdt, adt, bm, cm):
    b, L = xdt.shape[:2]
    nc = L // CHUNK

    def to_chunks(t):
        return jnp.moveaxis(t.reshape((b, nc, CHUNK) + t.shape[2:]), 1, 0)

    causal = jnp.tril(jnp.ones((CHUNK, CHUNK), dtype=bool))[None, :, :, None, None]

    def step(state, inp):
        xc, ac, bc, cc = inp
        acum = jnp.cumsum(ac.astype(jnp.float32), axis=1)
        seg = acum[:, :, None] - acum[:, None, :]
        decay = jnp.exp(jnp.where(causal, seg, -jnp.inf))
        cb = jnp.einsum('blgn,bsgn->blsg', cc, bc).astype(jnp.float32)
        y_diag = jnp.einsum('blsgr,bsgrp->blgrp', cb[..., None] * decay, xc)
        y_off = jnp.einsum('blgn,bgrpn->blgrp', cc, state) * jnp.exp(acum)[..., None]
        to_end = jnp.exp(acum[:, -1:] - acum)
        new_state = (state * jnp.exp(acum[:, -1])[..., None, None]
                     + jnp.einsum('blgn,blgr,blgrp->bgrpn', bc, to_end, xc))
        return new_state, y_diag + y_off

    state0 = jnp.zeros((b, SSD_N_GROUPS, SSD_HEADS_PER_GROUP, SSD_HEAD_DIM, SSD_D_STATE), jnp.float32)
    _, y = lax.scan(step, state0, (to_chunks(xdt), to_chunks(adt), to_chunks(bm), to_chunks(cm)))
    return jnp.moveaxis(y, 0, 1).reshape((b, L) + xdt.shape[2:])


def ssd_branch(z, xs, bs, cs, dt, valid, conv_w, conv_b, dt_bias, a_log, d_skip, norm_g, w_out):
    b, L, _ = xs.shape
    xbc = jnp.concatenate([xs, bs, cs], axis=-1)
    xbc = jax.nn.silu(causal_dwconv(xbc, conv_w) + conv_b) * valid
    xs = xbc[..., :SSD_D_INNER]
    bs = xbc[..., SSD_D_INNER:SSD_D_INNER + SSD_N_GROUPS * SSD_D_STATE]
    cs = xbc[..., SSD_D_INNER + SSD_N_GROUPS * SSD_D_STATE:]
    dt = jax.nn.softplus(dt.astype(jnp.float32) + dt_bias)
    a = -jnp.exp(a_log.astype(jnp.float32))
    xh = xs.reshape(b, L, SSD_N_GROUPS, SSD_HEADS_PER_GROUP, SSD_HEAD_DIM)
    dtg = dt.reshape(b, L, SSD_N_GROUPS, SSD_HEADS_PER_GROUP)
    y = ssd_chunked_scan(xh * dtg[..., None],
                         dtg * a.reshape(SSD_N_GROUPS, SSD_HEADS_PER_GROUP),
                         bs.reshape(b, L, SSD_N_GROUPS, SSD_D_STATE),
                         cs.reshape(b, L, SSD_N_GROUPS, SSD_D_STATE))
    y = y + xh * d_skip.reshape(SSD_N_GROUPS, SSD_HEADS_PER_GROUP)[..., None]
    y = y.reshape(b, L, SSD_D_INNER)
    y = group_rmsnorm(y * jax.nn.silu(z.astype(jnp.float32)), norm_g, SSD_N_GROUPS)
    return y.astype(xs.dtype) @ w_out


def shortconv_branch(sb, sc, sx, conv_w, w_out):
    return (sb * causal_dwconv(sc * sx, conv_w)) @ w_out


def peer_ffn(h, w_q, sub_keys, expert_u, expert_v):
    T, D = h.shape
    blocks = h.reshape(T // PEER_BLOCK, PEER_BLOCK, D)

    def one(xb):
        q = (xb @ w_q).reshape(PEER_BLOCK, PEER_HEADS, 2, PEER_HALF)
        s = jnp.einsum('thjd,hjkd->thjk', q, sub_keys).astype(jnp.float32)
        sv, si = lax.top_k(s, PEER_TOPK)
        cand = sv[:, :, 0, :, None] + sv[:, :, 1, None, :]
        cs, ci = lax.top_k(cand.reshape(PEER_BLOCK, PEER_HEADS, PEER_TOPK * PEER_TOPK), PEER_TOPK)
        i1 = jnp.take_along_axis(si[:, :, 0], ci // PEER_TOPK, axis=-1)
        i2 = jnp.take_along_axis(si[:, :, 1], ci % PEER_TOPK, axis=-1)
        eidx = i1 * PEER_N_KEYS + i2
        g = jax.nn.softmax(cs, axis=-1)
        u = expert_u[eidx]
        v = expert_v[eidx]
        act = jax.nn.gelu(jnp.einsum('td,thkd->thk', xb, u).astype(jnp.float32), approximate=False)
        return jnp.einsum('thk,thkd->td', (g * act).astype(v.dtype), v)

    return lax.map(one, blocks).reshape(T, D)


def setup_inputs(seed: int = 0) -> dict:
    key = jax.random.key(seed)
    ks = jax.random.split(key, 22)
    f32 = jnp.float32

    def nrm(k, shape, scale):
        return jax.random.normal(k, shape, f32) * scale

    x = nrm(ks[0], (BATCH, SEQ, D_MODEL), 1.0)
    meta_tokens = nrm(ks[1], (N_META, D_MODEL), 1.0)
    ln_mix = 1.0 + nrm(ks[2], (DEPTH, D_MODEL), 0.02)
    w_in = nrm(ks[3], (DEPTH, D_MODEL, PROJ_TOTAL), D_MODEL ** -0.5)
    ssd_conv_w = nrm(ks[4], (DEPTH, SSD_CONV_DIM, SSD_CONV_W), SSD_CONV_W ** -0.5)
    ssd_conv_b = nrm(ks[5], (DEPTH, SSD_CONV_DIM), 0.01)
    dt0 = jnp.exp(jax.random.uniform(ks[6], (DEPTH, SSD_N_HEADS), f32, math.log(1e-3), math.log(1e-1)))
    ssd_dt_bias = dt0 + jnp.log(-jnp.expm1(-dt0))
    ssd_a_log = jnp.log(jax.random.uniform(ks[7], (DEPTH, SSD_N_HEADS), f32, 1.0, 16.0))
    ssd_d = 1.0 + nrm(ks[8], (DEPTH, SSD_N_HEADS), 0.02)
    ssd_norm = 1.0 + nrm(ks[9], (DEPTH, SSD_D_INNER), 0.02)
    ssd_w_out = nrm(ks[10], (DEPTH, SSD_D_INNER, D_MODEL), SSD_D_INNER ** -0.5)
    sc_conv_w = nrm(ks[11], (DEPTH, SC_WIDTH, SC_CONV_W), SC_CONV_W ** -0.5)
    sc_w_out = nrm(ks[12], (DEPTH, SC_WIDTH, D_MODEL), SC_WIDTH ** -0.5)
    w_o = nrm(ks[13], (DEPTH, D_MODEL, D_MODEL), D_MODEL ** -0.5)
    ln_ffn = 1.0 + nrm(ks[14], (DEPTH, D_MODEL), 0.02)
    peer_w_q = nrm(ks[15], (DEPTH, D_MODEL, PEER_HEADS * PEER_D_KEY), D_MODEL ** -0.5)
    peer_sub_keys = nrm(ks[16], (DEPTH, PEER_HEADS, 2, PEER_N_KEYS, PEER_HALF), PEER_HALF ** -0.5)
    peer_u = nrm(ks[17], (DEPTH, PEER_N_EXPERTS, D_MODEL), D_MODEL ** -0.5)
    peer_v = nrm(ks[18], (DEPTH, PEER_N_EXPERTS, D_MODEL), PEER_HEADS ** -0.5)
    ln_final = 1.0 + nrm(ks[19], (D_MODEL,), 0.02)
    return {"x": x, "meta_tokens": meta_tokens, "ln_mix": ln_mix, "w_in": w_in,
            "ssd_conv_w": ssd_conv_w, "ssd_conv_b": ssd_conv_b, "ssd_dt_bias": ssd_dt_bias,
            "ssd_a_log": ssd_a_log, "ssd_d": ssd_d, "ssd_norm": ssd_norm, "ssd_w_out": ssd_w_out,
            "sc_conv_w": sc_conv_w, "sc_w_out": sc_w_out, "w_o": w_o, "ln_ffn": ln_ffn,
            "peer_w_q": peer_w_q, "peer_sub_keys": peer_sub_keys, "peer_u": peer_u,
            "peer_v": peer_v, "ln_final": ln_final}


def reference(x, meta_tokens, ln_mix, w_in, ssd_conv_w, ssd_conv_b, ssd_dt_bias, ssd_a_log, ssd_d,
              ssd_norm, ssd_w_out, sc_conv_w, sc_w_out, w_o, ln_ffn, peer_w_q, peer_sub_keys,
              peer_u, peer_v, ln_final):
    b = x.shape[0]
    pad = jnp.zeros((b, N_PAD, D_MODEL), x.dtype)
    meta = jnp.broadcast_to(meta_tokens[None].astype(x.dtype), (b, N_META, D_MODEL))
    h = jnp.concatenate([pad, meta, x], axis=1)
    lp = h.shape[1]
    valid = (jnp.arange(lp) >= N_PAD)[None, :, None].astype(x.dtype)
    split_idx = [int(c) for c in np.cumsum(PROJ_SIZES)[:-1]]

    for l in range(DEPTH):
        u = rmsnorm(h, ln_mix[l]) * valid
        proj = u @ w_in[l]
        z, xs, bs, cs, dt, sb, sc, sx, gates = jnp.split(proj, split_idx, axis=-1)
        y_ssd = ssd_branch(z, xs, bs, cs, dt, valid, ssd_conv_w[l], ssd_conv_b[l], ssd_dt_bias[l],
                           ssd_a_log[l], ssd_d[l], ssd_norm[l], ssd_w_out[l])
        y_sc = shortconv_branch(sb, sc, sx, sc_conv_w[l], sc_w_out[l])
        g = jax.nn.sigmoid(gates.astype(jnp.float32))
        merged = (g[..., :D_MODEL] * y_ssd + g[..., D_MODEL:] * y_sc).astype(x.dtype)
        h = h + merged @ w_o[l]
        u = rmsnorm(h, ln_ffn[l]) * valid
        ff = peer_ffn(u.reshape(-1, D_MODEL), peer_w_q[l], peer_sub_keys[l], peer_u[l], peer_v[l])
        h = h + ff.reshape(h.shape).astype(h.dtype)

    out = rmsnorm(h[:, N_PAD + N_META:], ln_final)
    return out
```

```python
import numpy as np
from contextlib import ExitStack
import concourse.bass as bass
import concourse.mybir as mybir
from concourse.bass_utils import run_bass_kernel_spmd

F32 = mybir.dt.float32
F32R = mybir.dt.float32r
BF16 = mybir.dt.bfloat16
U32 = mybir.dt.uint32
ALU = mybir.AluOpType
AF = mybir.ActivationFunctionType
AX = mybir.AxisListType

D = 1024
SEQ = 4096
TT = 128
NT_FULL = SEQ // TT + 1
NTOK_EXT = NT_FULL * TT
EPS = 1e-6
NEG = -1.0e5
NBLK = 68
RW = 3
RG = 5
DVE_SHARE = 3
PIPE_RATIO = 2
NROW = 2048 + 96


class Buf:
    def __init__(self, name, t):
        self.name = name
        self.t = t
        self.w = {}
        self.r = {}
        self.dsem = None
        self.dcnt = 0


class Multi:
    def __init__(self, t, parts):
        self.t = t
        self.parts = parts


class Sched:
    ENG = ("pe", "dve", "act", "pool", "sp")

    def __init__(self, nc, es):
        self.nc = nc
        self.es = es
        self.ops = {e: [] for e in self.ENG}
        self.cnt = {e: 0 for e in self.ENG}
        self.esem = {e: es.enter_context(nc.semaphore("sem_" + e)) for e in ("pe", "dve", "act", "pool")}
        self.waited = {e: {} for e in self.ENG}
        self.final = {}
        self.nins = 0

    def _deps(self, reads, writes):
        deps = {}

        def add(d):
            for k, sv in d.items():
                if k not in deps or deps[k][1] < sv[1]:
                    deps[k] = sv
        for b in reads:
            add(b.w)
        for b in writes:
            add(b.w)
            add(b.r)
        return deps

    def _waits(self, eng, deps):
        for k, (s, v) in deps.items():
            if eng == "pe" and k == "pe":
                continue
            if self.waited[eng].get(k, 0) < v:
                self.waited[eng][k] = v
                self.ops[eng].append(("wait", s, v))

    def _update(self, key, ev, reads, writes):
        for b in reads:
            if b in writes:
                continue
            cur = b.r.get(key)
            if cur is None or cur[1] < ev[1]:
                b.r[key] = ev
        for b in writes:
            if b.r:
                b.w = {key: ev}
                b.r = {}
            else:
                cur = b.w.get(key)
                if cur is None or cur[1] < ev[1]:
                    b.w[key] = ev

    @staticmethod
    def _flat(bufs):
        out = []
        for b in bufs:
            if isinstance(b, Multi):
                out.extend(b.parts)
            else:
                out.append(b)
        return out

    def op(self, eng, fn, reads=(), writes=()):
        reads = self._flat(reads); writes = self._flat(writes)
        self._waits(eng, self._deps(reads, writes))
        self.cnt[eng] += 1
        ev = (self.esem[eng], self.cnt[eng])
        self.ops[eng].append(("ins", fn, self.esem[eng], 1))
        self._update(eng, ev, reads, writes)
        self.nins += 1

    def dma(self, eng, fn, sembuf, reads=(), writes=(), is_out=False):
        reads = self._flat(reads); writes = self._flat(writes)
        self._waits(eng, self._deps(reads, writes))
        if sembuf.dsem is None:
            sembuf.dsem = self.es.enter_context(self.nc.semaphore("dsem_" + sembuf.name))
        sembuf.dcnt += 16
        key = "d_" + sembuf.name
        ev = (sembuf.dsem, sembuf.dcnt)
        self.ops[eng].append(("ins", fn, sembuf.dsem, 16))
        self._update(key, ev, reads, writes)
        self.final[key] = ev
        self.nins += 1

    def replay(self):
        import os as _os
        for e in ("pe", "dve", "act", "pool"):
            if self.cnt[e] > 0 and _os.environ.get("FINAL_ALL", "1") == "1":
                self.final[e] = (self.esem[e], self.cnt[e])
        for k, (s, v) in self.final.items():
            if self.waited["sp"].get(k, 0) < v:
                self.ops["sp"].append(("wait", s, v))
        ops = self.ops

        allsems = [sv[0] for sv in self.final.values()]

        def mk(e):
            def body(engobj):
                for item in ops[e]:
                    if item[0] == "wait":
                        engobj.wait_ge(item[1], item[2])
                    else:
                        item[1](engobj).then_inc(item[2], item[3])
            return body
        with self.nc.Block() as block:
            block.tensor(mk("pe"))
            block.vector(mk("dve"))
            block.scalar(mk("act"))
            block.gpsimd(mk("pool"))
            block.sync(mk("sp"))


def rr2(gens, width=2, offset=4):
    active = []
    it = iter(gens)
    since = offset
    while True:
        if len(active) < width and (since >= offset or not active):
            g = next(it, None)
            if g is not None:
                active.append(g)
                since = 0
        if not active:
            return
        for g in list(active):
            try:
                next(g)
            except StopIteration:
                active.remove(g)
            else:
                yield
        since += 1


class _Stop(Exception):
    pass


def build(NT=NT_FULL, with_peer=True, taps=(), maxstage=99):
    nc = bass.Bass("TRN2", target_bir_lowering=False)
    es = ExitStack()
    S = Sched(nc, es)

    def stage(n):
        if n > maxstage:
            raise _Stop()

    def dram_in(name, shape, dt=F32):
        return nc.dram_tensor(name, list(shape), dt, kind="ExternalInput").ap()

    xh = dram_in("xh", [NTOK_EXT, D])
    wall = dram_in("wall", [NBLK, 128, 2048])
    uv = dram_in("uv", [16384, 2048])
    d_ident = dram_in("c_ident", [128, 128])
    d_tri = dram_in("c_tri", [128, 128])
    d_negm = dram_in("c_negm", [128, 128])
    d_ones = dram_in("c_ones", [128, 128])
    d_iota = dram_in("c_iota", [128, 16])
    d_rows = dram_in("c_rows", [128, NROW])
    d_cw = dram_in("c_cw", [128, 32 * 5])
    d_scw = dram_in("c_scw", [128, 8 * 3])
    d_wdt = dram_in("c_wdt", [128, 8 * 32])
    d_skt = dram_in("c_skt", [128, 16 * 128])
    d_lnmix = dram_in("c_lnmix", [128, 8])
    d_ngc = dram_in("c_ngc", [128, 16])
    n_out_rows = max((NT - 1) * TT, TT)
    out = nc.dram_tensor("out", [n_out_rows, D], F32, kind="ExternalOutput").ap()
    tap_out = {}
    for (nm, tj, words) in taps:
        tap_out[(nm, tj)] = nc.dram_tensor("tap_%s_%d" % (nm, tj), [128, words], F32, kind="ExternalOutput").ap()

    uvb = nc.dram_tensor("uvb", [16384, 2048], BF16, kind="Internal").ap()
    UVB = Buf("UVB", None)

    def sb(name, words, dt=F32):
        t = es.enter_context(nc.sbuf_tensor(name, [128, words], dt))
        return Buf(name, t)

    def ps(name):
        t = es.enter_context(nc.psum_tensor(name, [128, 512], F32))
        return Buf(name, t)

    ident = sb("ident", 128); tri = sb("tri", 128); negm = sb("negm", 128); ones = sb("ones", 128)
    iota16 = sb("iota16", 16); rows = sb("rows", NROW); cw = sb("cw", 160); scw = sb("scw", 24)
    wdt = sb("wdt", 256); skt = sb("skt", 2048); lnmix = sb("lnmix", 8); ngc = sb("ngc", 16)
    for b_, d_ in ((ident, d_ident), (tri, d_tri), (negm, d_negm), (ones, d_ones), (iota16, d_iota),
                   (rows, d_rows), (cw, d_cw), (scw, d_scw), (wdt, d_wdt), (skt, d_skt),
                   (lnmix, d_lnmix), (ngc, d_ngc)):
        S.dma("sp", (lambda e, o=b_.t[:], i=d_: e.dma_start(o, i)), b_, writes=[b_])
    LNF = rows.t[:, 0:1024]; LNO = rows.t[:, 1024:2048]
    DTB = rows.t[:, 2048:2080]; ALOG = rows.t[:, 2080:2112]; DSK = rows.t[:, 2112:2144]

    P = [ps("ps%d" % i) for i in range(8)]
    pctr = [0]

    FFP = [P[6], P[7]]

    def pnext():
        b = P[pctr[0] % 6]
        pctr[0] += 1
        return b

    identb = Buf("identb", es.enter_context(nc.sbuf_tensor("identb", [128, 128], BF16)))
    DGr = [Buf("DG%d" % i, es.enter_context(nc.sbuf_tensor("DG%d" % i, [128, 128], BF16))) for i in range(3)]

    Hr = [sb("H%d" % i, 1024) for i in range(2)]
    Ub = sb("U", 1024); UT = sb("UT", 1024)
    Wr = [sb("W%d" % i, 2048) for i in range(2)]
    Wc = [sb("Wc%d" % i, 2048, F32R) for i in range(2)]
    XR = [sb("XR%d" % i, 4 * 131) for i in range(2)]
    XC = [sb("XC%d" % i, 512) for i in range(2)]
    HIST = [sb("HIST%d" % g, 12) for g in range(8)]
    ST = [sb("ST%d" % g, 256) for g in range(8)]
    YNT = sb("YNT", 2048)
    Gb = [sb("G%d" % i, 256) for i in range(2)]
    VMG = sb("VMG", 1024)
    Vb = VMG; MG = Ub; MGT = sb("MGT", 1024)
    PH = [sb("PH%d" % c, 130) for c in range(8)]
    SCs = sb("SCs", 256); CACC = sb("CACC", 128)
    GT = []
    for par in range(2):
        GT.append(dict(
            zs=sb("zs%d" % par, 256), XDT=sb("XDT%d" % par, 256), XDW=sb("XDW%d" % par, 256), XSD=sb("XSD%d" % par, 256),
            BTs=sb("BTs%d" % par, 128), CBm=sb("CBm%d" % par, 128), SEG=sb("SEG%d" % par, 512),
            DEC=sb("DEC%d" % par, 512), MT=sb("MT%d" % par, 512), Y1=sb("Y1%d" % par, 256), Y2=sb("Y2%d" % par, 256),
            YZ=sb("YZ%d" % par, 256), YN=sb("YN%d" % par, 256), ACC=sb("ACC%d" % par, 128),
            ADTB=sb("ADTB%d" % par, 512), ss2=sb("ss2%d" % par, 1), rstd2=sb("rstd2%d" % par, 1)))
    TMr = [sb("TM%d" % i, 512) for i in range(2)]
    tmc = [0]

    def tmnext():
        b = TMr[tmc[0] % 2]
        tmc[0] += 1
        return b
    small = {}
    GBt = [es.enter_context(nc.sbuf_tensor("GBt%d" % i, [128, 2048], F32)) for i in range(3)]
    Gh = [Buf("Gh%d" % k, GBt[k // 2]) for k in range(6)]
    Gfull = [Multi(GBt[i], [Gh[2 * i], Gh[2 * i + 1]]) for i in range(3)]

    def gslot(k):
        return GBt[k // 2][:].bitcast(BF16)[:, (k % 2) * 2048:(k % 2 + 1) * 2048]

    def sm(name, words=32, dt=F32):
        if name not in small:
            small[name] = sb(name, words, dt)
        return small[name]

    aneg = sm("aneg"); ss = sm("ss", 1); rstd = sm("rstd", 1)

    def tt(out_, in0, in1, op, r, w, eng="dve"):
        S.op(eng, lambda e: e.tensor_tensor(out_, in0, in1, op), r, w)

    def ts(out_, in0, s1, s2, op0, op1, r, w, eng="dve"):
        if s2 is None:
            S.op(eng, lambda e: e.tensor_scalar(out_, in0, s1, None, op0), r, w)
        else:
            S.op(eng, lambda e: e.tensor_scalar(out_, in0, s1, s2, op0, op1), r, w)

    def stt(out_, in0, scalar, in1, op0, op1, r, w):
        S.op("dve", lambda e: e.scalar_tensor_tensor(out_, in0, scalar, in1, op0, op1), r, w)

    zero = sb("zero", 1)

    def act(out_, in_, func, r, w, **kw):
        if "bias" not in kw:
            kw["bias"] = zero.t[:, 0:1]
            r = list(r) + [zero]
        S.op("act", lambda e: e.activation(out_, in_, func, **kw), r, w)

    def cp(out_, in_, r, w, eng="act"):
        if eng == "act":
            S.op("act", lambda e: e.copy(out_, in_), r, w)
        else:
            S.op(eng, lambda e: e.tensor_copy(out_, in_), r, w)

    def mm(out_, lhsT, rhs, start, stop, r, w):
        S.op("pe", lambda e: e.matmul(out_, lhsT, rhs, start=start, stop=stop), r, w)

    def mmr(out_, lhsT, rhs, start, stop, r, w):
        S.op("pe", lambda e: e.matmul(out_, lhsT.bitcast(F32R), rhs.bitcast(F32R), start=start, stop=stop), r, w)

    def tr(out_, in_, r, w):
        S.op("pe", lambda e: e.transpose(out_, in_, ident.t[:]), list(r) + [ident], w)

    def memset(ap, val, w, eng="dve"):
        S.op(eng, lambda e: e.memset(ap, val), (), w)

    def tap(name, j, buf, ap, words):
        if (name, j) in tap_out:
            S.dma("sp", (lambda e, o=tap_out[(name, j)], i=ap: e.dma_start(o, i)), buf, reads=[buf], is_out=True)

    req = []
    wpos = [0]
    wloaded = [0]
    mq = {"m": 0, "q": 0}

    def wload():
        p = wloaded[0]
        slot = Wr[p % 2]
        S.dma("sp", (lambda e, o=slot.t[:], p=p: e.dma_start(o, wall[req[p] if p < len(req) else 0])), slot,
              writes=[slot])
        wloaded[0] += 1

    def wget(exact=False):
        if exact:
            req.append(60 + mq["q"] % 8)
            mq["q"] += 1
        else:
            req.append(mq["m"] % 60)
            mq["m"] += 1
        st = Wr[wpos[0] % 2]
        if exact:
            return st, st.t[:].rearrange("p (k c) -> p k c", c=256)
        wc = Wc[wpos[0] % 2]
        cp(wc.t[:], st.t[:], [st], [wc])
        return wc, wc.t[:].rearrange("p (k c) -> p k c", c=256)

    def wdone():
        wpos[0] += 1
        wload()

    for _ in range(2):
        wload()

    def init_ops():
        memset(zero.t[:], 0.0, [zero])
        cp(identb.t[:], ident.t[:], [ident], [identb], eng="dve")
        stage(0.2)
        act(aneg.t[:, 0:32], ALOG, AF.Exp, [rows], [aneg])
        ts(aneg.t[:, 0:32], aneg.t[:, 0:32], -1.0, None, ALU.mult, None, [aneg], [aneg])
        stage(0.3)
        for g in range(8):
            memset(HIST[g].t[:], 0.0, [HIST[g]], eng="pool")
            memset(ST[g].t[:], 0.0, [ST[g]], eng="pool")
        for c in range(8):
            memset(PH[c].t[:], 0.0, [PH[c]], eng="pool")
        stage(0.4)


    def rmsnorm_stats(src, junk, ssb, rsb):
        act(junk.t[:, 0:1024], src.t[:, 0:1024], AF.Square, [src], [junk, ssb], accum_out=ssb.t[:, 0:1])
        ts(ssb.t[:, 0:1], ssb.t[:, 0:1], 1.0 / 1024.0, EPS, ALU.mult, ALU.add, [ssb], [ssb])
        act(ssb.t[:, 0:1], ssb.t[:, 0:1], AF.Sqrt, [ssb], [ssb])
        S.op("dve", lambda e: e.reciprocal(rsb.t[:, 0:1], ssb.t[:, 0:1]), [ssb], [rsb])

    def transpose8(src, dst, scale_col=None, r32=False):
        for half in range(2):
            pb = pnext()
            for q in range(4):
                kt = half * 4 + q
                tr(pb.t[:, q * 128:(q + 1) * 128], src.t[:, kt * 128:(kt + 1) * 128], [src], [pb])
            o = dst.t[:, half * 512:(half + 1) * 512]
            if r32:
                o = o.bitcast(F32R)
            if scale_col is None:
                cp(o, pb.t[:, 0:512], [pb], [dst], eng="dve")
            else:
                sc_b, sc_ap = scale_col
                tt(o.rearrange("p (k t) -> p k t", t=128), pb.t[:, 0:512].rearrange("p (k t) -> p k t", t=128),
                   sc_ap[:, half * 4:(half + 1) * 4].unsqueeze(2).to_broadcast([128, 4, 128]), ALU.mult,
                   [pb, sc_b], [dst])

    UT3 = UT.t[:].rearrange("p (k t) -> p k t", t=128)
    YNT3 = YNT.t[:].rearrange("p (k t) -> p k t", t=128)
    MGT3 = MGT.t[:].rearrange("p (k t) -> p k t", t=128)
    wdt3 = wdt.t[:].rearrange("p (k c) -> p k c", c=32)
    cw3 = cw.t[:].rearrange("p (c k) -> p c k", k=5)
    scw3 = scw.t[:].rearrange("p (c k) -> p c k", k=3)

    PHS = {"ratio": 1, "dve_acc": False}
    ENV = dict(sm=sm, tt=tt, ts=ts, stt=stt, act=act, cp=cp, mm=mm, pnext=pnext, wget=wget, wdone=wdone,
               memset=memset, tap=tap, rows=rows, LNF=LNF, skt=skt, iota16=iota16, uv=uv,
               transpose8=transpose8, rmsnorm_stats=rmsnorm_stats, YNT=YNT, VMG=VMG, MGT=MGT,
               Gh=Gh, Gfull=Gfull, gslot=gslot, uvb=uvb, UVB=UVB, FFP=FFP, identb=identb, DGr=DGr, PHS=PHS)

    def mixer(j):
        PHS["ratio"] = 1
        PHS["dve_acc"] = False
        H = Hr[j % 2]
        S.dma("sp", (lambda e, o=H.t[:], i=xh[j * TT:(j + 1) * TT, :]: e.dma_start(o, i)), H, writes=[H])
        stage(0.5)
        rmsnorm_stats(H, Ub, ss, rstd)
        stage(0.6)
        ts(Ub.t[:], H.t[:], rstd.t[:, 0:1], None, ALU.mult, None, [H, rstd], [Ub])
        tap("U", j, Ub, Ub.t[:], 1024)
        stage(0.7)
        transpose8(Ub, UT, scale_col=(lnmix, lnmix.t), r32=True)
        tap("UT", j, UT, UT.t[:], 1024)
        stage(1)
        yield

        dtp = pnext()
        for kt in range(8):
            mm(dtp.t[:, 0:32], UT3[:, kt, :], wdt3[:, kt, :], kt == 0, kt == 7, [UT, wdt], [dtp])
        xd = sm("xd"); ax = sm("ax"); ex = sm("ex"); dtb = sm("dt"); adt = sm("adt")
        acum = sm("acum"); Eb = sm("E"); wb = sm("w"); eA = sm("eA"); dif = sm("dif")
        tt(xd.t[:], dtp.t[:, 0:32], DTB, ALU.add, [dtp, rows], [xd])
        stage(1.2)
        stt(ax.t[:], xd.t[:], -1.0, xd.t[:], ALU.mult, ALU.max, [xd], [ax])
        act(ex.t[:], ax.t[:], AF.Exp, [ax], [ex], scale=-1.0)
        stage(1.4)
        ts(ex.t[:], ex.t[:], 1.0, None, ALU.add, None, [ex], [ex])
        act(ex.t[:], ex.t[:], AF.Ln, [ex], [ex])
        stage(1.5)
        stt(dtb.t[:], xd.t[:], 0.0, ex.t[:], ALU.max, ALU.add, [xd, ex], [dtb])
        tt(adt.t[:], dtb.t[:], aneg.t[:], ALU.mult, [dtb, aneg], [adt])
        tap("dt", j, dtb, dtb.t[:], 32)
        stage(1.6)
        cp_ = pnext()
        mm(cp_.t[:, 0:32], tri.t[:], adt.t[:], True, True, [tri, adt], [cp_])
        mm(cp_.t[:, 32:64], ones.t[:], adt.t[:], True, True, [ones, adt], [cp_])
        stage(1.7)
        cp(acum.t[:], cp_.t[:, 0:32], [cp_], [acum], eng="dve")
        act(Eb.t[:], cp_.t[:, 0:32], AF.Exp, [cp_], [Eb])
        act(eA.t[:], cp_.t[:, 32:64], AF.Exp, [cp_], [eA])
        stage(1.8)
        tt(dif.t[:], cp_.t[:, 32:64], acum.t[:], ALU.subtract, [cp_, acum], [dif])
        act(wb.t[:], dif.t[:], AF.Exp, [dif], [wb])
        stage(2)
        yield

        def group(g):
            T = GT[g % 2]
            zs = T['zs']; XDT = T['XDT']; XDW = T['XDW']; XSD = T['XSD']; BTs = T['BTs']; CBm = T['CBm']
            SEG = T['SEG']; DEC = T['DEC']; MT = T['MT']; Y1 = T['Y1']; Y2 = T['Y2']; YZ = T['YZ']; YN = T['YN']
            ACC = T['ACC']; ADTB = T['ADTB']; ss2 = T['ss2']; rstd2 = T['rstd2']
            xr = XR[g % 2]; xc = XC[g % 2]
            xr3 = xr.t[:].rearrange("p (c t) -> p c t", t=131)
            xc3 = xc.t[:].rearrange("p (c t) -> p c t", t=128)
            wbuf, w3 = wget()
            zp = pnext()
            for kt in range(8):
                mmr(zp.t[:, 0:256], UT3[:, kt, :], w3[:, kt, :], kt == 0, kt == 7, [UT, wbuf], [zp])
            wdone()
            stage(2.05)
            act(zs.t[:], zp.t[:, 0:256], AF.Silu, [zp], [zs])
            stage(2.1)
            tmq = pnext()
            for half in range(2):
                wbuf, w3 = wget()
                for kt in range(8):
                    mmr(tmq.t[:, half * 256:(half + 1) * 256], UT3[:, kt, :], w3[:, kt, :], kt == 0, kt == 7,
                        [UT, wbuf], [tmq])
                wdone()
            TM = tmnext()
            cp(TM.t[:], tmq.t[:, 0:512], [tmq], [TM])
            xp = pnext()
            for i in range(4):
                tr(xp.t[:, i * 128:(i + 1) * 128], TM.t[:, i * 128:(i + 1) * 128], [TM], [xp])
            stage(2.2)
            cp(xr3[:, :, 0:3], HIST[g].t[:].rearrange("p (c t) -> p c t", t=3), [HIST[g]], [xr], eng="pool")
            stage(2.25)
            cp(xr3[:, :, 3:131], xp.t[:, 0:512].rearrange("p (c t) -> p c t", t=128), [xp], [xr])
            cp(HIST[g].t[:].rearrange("p (c t) -> p c t", t=3), xr3[:, :, 128:131], [xr], [HIST[g]], eng="pool")
            stage(2.3)
            for i in range(4):
                c = 4 * g + i
                ts(ACC.t[:], xr3[:, i, 0:128], cw3[:, c, 0:1], None, ALU.mult, None, [xr, cw], [ACC])
                for k in range(1, 4):
                    stt(ACC.t[:], xr3[:, i, k:k + 128], cw3[:, c, k:k + 1], ACC.t[:], ALU.mult, ALU.add,
                        [xr, cw, ACC], [ACC])
                stage(2.4)
                act(xc3[:, i, :], ACC.t[:], AF.Silu, [ACC, cw], [xc], bias=cw3[:, c, 4:5])
                stage(2.5)
            if j == 0:
                memset(xc3[:, :, 0:112], 0.0, [xc])
            if g == 0:
                tap("xc", j, xc, xc.t[:], 512)
            stage(3)
            yield
            tp = pnext()
            for i in range(3):
                tr(tp.t[:, i * 128:(i + 1) * 128], xc3[:, i, :], [xc], [tp])
            stage(3.1)
            hs = slice(4 * g, 4 * g + 4)
            tp3 = tp.t[:, 0:256].rearrange("p (r q) -> p r q", q=64)
            tt(XDT.t[:].rearrange("p (r q) -> p r q", q=64), tp3,
               dtb.t[:, hs].unsqueeze(2).to_broadcast([128, 4, 64]), ALU.mult, [tp, dtb], [XDT])
            tt(XSD.t[:].rearrange("p (r q) -> p r q", q=64), tp3,
               DSK[:, hs].unsqueeze(2).to_broadcast([128, 4, 64]), ALU.mult, [tp, rows], [XSD])
            stage(3.2)
            cp(BTs.t[:], tp.t[:, 256:384], [tp], [BTs], eng="dve")
            stage(3.25)
            tt(XDW.t[:].rearrange("p (r q) -> p r q", q=64), XDT.t[:].rearrange("p (r q) -> p r q", q=64),
               wb.t[:, hs].unsqueeze(2).to_broadcast([128, 4, 64]), ALU.mult, [XDT, wb], [XDW])
            stage(3.3)
            yield
            cbp = pnext()
            mm(cbp.t[:, 0:128], xc3[:, 2, :], xc3[:, 3, :], True, True, [xc], [cbp])
            tt(CBm.t[:], cbp.t[:, 0:128], tri.t[:], ALU.mult, [cbp, tri], [CBm])
            stage(3.4)
            yield
            rp = pnext()
            cp(ADTB.t[:].rearrange("p (r m) -> p r m", m=128),
               adt.t[:, 4 * g:4 * g + 4].unsqueeze(2).to_broadcast([128, 4, 128]), [adt], [ADTB], eng="dve")
            for r in range(4):
                h = 4 * g + r
                mm(rp.t[:, r * 128:(r + 1) * 128], ADTB.t[:, r * 128:(r + 1) * 128], tri.t[:],
                   True, True, [ADTB, tri], [rp])
            stage(3.5)
            for r in range(4):
                h = 4 * g + r
                stt(SEG.t[:, r * 128:(r + 1) * 128], rp.t[:, r * 128:(r + 1) * 128], acum.t[:, h:h + 1], negm.t[:],
                    ALU.subtract, ALU.add, [rp, acum, negm], [SEG])
            act(DEC.t[:], SEG.t[:], AF.Exp, [SEG], [DEC])
            stage(3.7)
            tt(MT.t[:].rearrange("p (r l) -> p r l", l=128), DEC.t[:].rearrange("p (r l) -> p r l", l=128),
               CBm.t[:].unsqueeze(1).to_broadcast([128, 4, 128]), ALU.mult, [DEC, CBm], [MT])
            stage(4)
            yield
            yp = pnext()
            for r in range(4):
                mm(yp.t[:, r * 64:(r + 1) * 64], MT.t[:, r * 128:(r + 1) * 128], XDT.t[:, r * 64:(r + 1) * 64],
                   True, True, [MT, XDT], [yp])
            mm(yp.t[:, 256:512], xc3[:, 3, :], ST[g].t[:], True, True, [xc, ST[g]], [yp])
            tt(Y1.t[:], yp.t[:, 0:256], XSD.t[:], ALU.add, [yp, XSD], [Y1])
            tt(Y2.t[:].rearrange("p (r q) -> p r q", q=64), yp.t[:, 256:512].rearrange("p (r q) -> p r q", q=64),
               Eb.t[:, hs].unsqueeze(2).to_broadcast([128, 4, 64]), ALU.mult, [yp, Eb], [Y2])
            tt(Y1.t[:], Y1.t[:], Y2.t[:], ALU.add, [Y1, Y2], [Y1])
            yield
            sp_ = pnext()
            mm(sp_.t[:, 0:256], BTs.t[:], XDW.t[:], True, True, [BTs, XDW], [sp_])
            tt(ST[g].t[:].rearrange("p (r q) -> p r q", q=64), ST[g].t[:].rearrange("p (r q) -> p r q", q=64),
               eA.t[:, hs].unsqueeze(2).to_broadcast([128, 4, 64]), ALU.mult, [ST[g], eA], [ST[g]])
            tt(ST[g].t[:], ST[g].t[:], sp_.t[:, 0:256], ALU.add, [ST[g], sp_], [ST[g]])
            yield
            tt(YZ.t[:], Y1.t[:], zs.t[:], ALU.mult, [Y1, zs], [YZ])
            act(YN.t[:], YZ.t[:], AF.Square, [YZ], [YN, ss2], accum_out=ss2.t[:, 0:1])
            ts(ss2.t[:, 0:1], ss2.t[:, 0:1], 1.0 / 256.0, EPS, ALU.mult, ALU.add, [ss2], [ss2])
            act(ss2.t[:, 0:1], ss2.t[:, 0:1], AF.Sqrt, [ss2], [ss2])
            S.op("dve", lambda e: e.reciprocal(rstd2.t[:, 0:1], ss2.t[:, 0:1]), [ss2], [rstd2])
            ts(YN.t[:], YZ.t[:], rstd2.t[:, 0:1], None, ALU.mult, None, [YZ, rstd2], [YN])
            if g == 0:
                tap("yn", j, YN, YN.t[:], 256)
            stage(5)
            yield
            np_ = pnext()
            for i in range(2):
                tr(np_.t[:, i * 128:(i + 1) * 128], YN.t[:, i * 128:(i + 1) * 128], [YN], [np_])
            tt(YNT3[:, 2 * g:2 * g + 2, :].bitcast(F32R), np_.t[:, 0:256].rearrange("p (k t) -> p k t", t=128),
               ngc.t[:, 2 * g:2 * g + 2].unsqueeze(2).to_broadcast([128, 2, 128]), ALU.mult, [np_, ngc], [YNT])
            yield

        yield from rr2([group(g) for g in range(8)])

        PHS["ratio"] = 3
        PHS["dve_acc"] = True
        V3 = Vb.t[:, 0:1024].rearrange("p (c t) -> p c t", t=128)
        for i in range(4):
            sbp = pnext(); scp = pnext(); sxp = pnext()
            for pb in (sbp, scp, sxp):
                tmq = pnext()
                wbuf, w3 = wget()
                for kt in range(8):
                    mmr(tmq.t[:, 0:256], UT3[:, kt, :], w3[:, kt, :], kt == 0, kt == 7, [UT, wbuf], [tmq])
                wdone()
                TM = tmnext()
                cp(TM.t[:, 0:256], tmq.t[:, 0:256], [tmq], [TM])
                for q in range(2):
                    tr(pb.t[:, q * 128:(q + 1) * 128], TM.t[:, q * 128:(q + 1) * 128], [TM], [pb])
                yield
            cp(SCs.t[:], scp.t[:, 0:256], [scp], [SCs])
            for q in range(2):
                c = 2 * i + q
                ph = PH[c]
                tt(ph.t[:, 2:130], sxp.t[:, q * 128:(q + 1) * 128], SCs.t[:, q * 128:(q + 1) * 128], ALU.mult,
                   [sxp, SCs], [ph])
                ts(CACC.t[:], ph.t[:, 0:128], scw3[:, c, 0:1], None, ALU.mult, None, [ph, scw], [CACC])
                for k in range(1, 3):
                    stt(CACC.t[:], ph.t[:, k:k + 128], scw3[:, c, k:k + 1], CACC.t[:], ALU.mult, ALU.add,
                        [ph, scw, CACC], [CACC])
                tt(V3[:, c, :].bitcast(F32R), sbp.t[:, q * 128:(q + 1) * 128], CACC.t[:], ALU.mult, [sbp, CACC], [Vb])
                cp(ph.t[:, 0:2], ph.t[:, 128:130], [ph], [ph], eng="pool")
            yield

        stage(6)
        for cb in range(4):
            cs_ = slice(cb * 256, (cb + 1) * 256)
            for gi in range(2):
                wbuf, w3 = wget()
                gp = pnext()
                for kt in range(8):
                    mmr(gp.t[:, 0:256], UT3[:, kt, :], w3[:, kt, :], kt == 0, kt == 7, [UT, wbuf], [gp])
                wdone()
                act(Gb[gi].t[:, 0:256], gp.t[:, 0:256], AF.Sigmoid, [gp], [Gb[gi]])
                yield
            yssd = pnext()
            for kh in range(2):
                wbuf, w3 = wget()
                for kt in range(8):
                    mmr(yssd.t[:, 0:256], YNT3[:, kh * 8 + kt, :], w3[:, kt, :], kh == 0 and kt == 0,
                       kh == 1 and kt == 7, [YNT, wbuf], [yssd])
                wdone()
            tt(MG.t[:, cs_], yssd.t[:, 0:256], Gb[0].t[:, 0:256], ALU.mult, [yssd, Gb[0]], [MG])
            yield
            ysc = pnext()
            wbuf, w3 = wget()
            for kt in range(8):
                mmr(ysc.t[:, 0:256], V3[:, kt, :], w3[:, kt, :], kt == 0, kt == 7, [Vb, wbuf], [ysc])
            wdone()
            tt(Gb[1].t[:, 0:256], ysc.t[:, 0:256], Gb[1].t[:, 0:256], ALU.mult, [ysc, Gb[1]], [Gb[1]])
            tt(MG.t[:, cs_], MG.t[:, cs_], Gb[1].t[:, 0:256], ALU.add, [MG, Gb[1]], [MG])
            yield
        transpose8(MG, MGT, r32=True)
        for cb in range(4):
            cs_ = slice(cb * 256, (cb + 1) * 256)
            wbuf, w3 = wget()
            op_ = pnext()
            for kt in range(8):
                mmr(op_.t[:, 0:256], MGT3[:, kt, :], w3[:, kt, :], kt == 0, kt == 7, [MGT, wbuf], [op_])
            wdone()
            tt(H.t[:, cs_], H.t[:, cs_], op_.t[:, 0:256], ALU.add, [H, op_], [H])
            yield
        tap("h2", j, H, H.t[:], 1024)


    def back(j, H, st):
        FF = sm("FF", 1024)
        if with_peer:
            yield from peer_back(nc, S, dict(ENV, j=j, H=H, FF=FF), st)
        else:
            memset(FF.t[:], 0.0, [FF], eng="pool")
        tt(FF.t[:], FF.t[:], H.t[:], ALU.add, [FF, H], [FF])
        ss3 = sm("ss3", 1); rstd3 = sm("rstd3", 1); junk3 = sm("JK", 1024)
        rmsnorm_stats(FF, junk3, ss3, rstd3)
        stt(FF.t[:], FF.t[:], rstd3.t[:, 0:1], LNO, ALU.mult, ALU.mult, [FF, rstd3, rows], [FF])
        S.dma("sp", (lambda e, o=out[(j - 1) * TT:j * TT, :], i=FF.t[:]: e.dma_start(o, i)), FF, reads=[FF],
              is_out=True)
        yield

    def tail(j):
        H = Hr[j % 2]
        st = {}
        if with_peer:
            yield from peer_front(nc, S, dict(ENV, j=j, H=H, FF=sm("FF", 1024)), st)
        yield from back(j, H, st)

    def convert_uv():
        outs = [sm("JK", 1024), sm("FF", 1024)]
        cvs = [Buf("cvs%d" % i, None) for i in range(3)]
        for c in range(128):
            stg = Gfull[c % 3]
            S.dma("sp", (lambda e, o=stg.t[:], i=uv[c * 128:(c + 1) * 128, :]: e.dma_start(o, i)), cvs[c % 3],
                  writes=[stg])
            ob = outs[c % 2]
            cp(ob.t[:].bitcast(BF16), stg.t[:], [stg], [ob], eng="pool")
            S.dma("sp", (lambda e, o=uvb[c * 128:(c + 1) * 128, :], i=ob.t[:].bitcast(BF16): e.dma_start(o, i)), ob,
                  reads=[ob], writes=[UVB])
            yield

    def drain(gen):
        if gen is not None:
            for _ in gen:
                pass

    def interleave(gm, gb, ratio):
        for _ in gm:
            if gb is not None:
                for _k in range(PHS["ratio"] if ratio else 0):
                    try:
                        next(gb)
                    except StopIteration:
                        gb = None
                        break
        drain(gb)

    try:
        init_ops()
        pending = convert_uv() if (with_peer and NT > 1) else None
        for j in range(NT):
            interleave(mixer(j), pending, PIPE_RATIO)
            pending = tail(j) if j >= 1 else None
        drain(pending)
    except _Stop:
        pass
    S.replay()
    es.close()
    return nc, es


def peer_front(nc, S, L, st):
    sm = L["sm"]; tt = L["tt"]; ts = L["ts"]; stt = L["stt"]; act = L["act"]; cp = L["cp"]; mm = L["mm"]
    pnext = L["pnext"]; wget = L["wget"]; wdone = L["wdone"]; memset = L["memset"]; tap = L["tap"]; j = L["j"]
    H = L["H"]; rows = L["rows"]; LNF = L["LNF"]; skt = L["skt"]; iota16 = L["iota16"]; uv = L["uv"]
    transpose8 = L["transpose8"]; rmsnorm_stats = L["rmsnorm_stats"]
    Gfull = L["Gfull"]
    QT = Gfull[0]; SCO = Gfull[1]; U2T = sm("U2T", 1024); FF = L["FF"]
    U2 = sm("U2", 1024); ssp = sm("ssp", 1); rsp = sm("rsp", 1)
    rmsnorm_stats(H, FF, ssp, rsp)
    stt(U2.t[:], H.t[:], rsp.t[:, 0:1], LNF, ALU.mult, ALU.mult, [H, rsp, rows], [U2])
    transpose8(U2, U2T)
    U2T3 = U2T.t[:].rearrange("p (k t) -> p k t", t=128)
    QT3 = QT.t[:].rearrange("p (c t) -> p c t", t=128)
    skt3 = skt.t[:].rearrange("p (c k) -> p c k", k=128)
    for i in range(8):
        wbuf, w3 = wget(exact=True)
        qp = pnext()
        for q in range(2):
            for kt in range(8):
                mm(qp.t[:, q * 128:(q + 1) * 128], w3[:, kt, q * 128:(q + 1) * 128], U2T3[:, kt, :],
                   kt == 0, kt == 7, [U2T, wbuf], [qp])
        wdone()
        cp(QT.t[:, i * 256:(i + 1) * 256], qp.t[:, 0:256], [qp], [QT])
        yield
    SCO3 = SCO.t[:].rearrange("p (c k) -> p c k", k=128)
    for b4 in range(4):
        sp_ = pnext()
        for q in range(4):
            c = b4 * 4 + q
            mm(sp_.t[:, q * 128:(q + 1) * 128], QT3[:, c, :], skt3[:, c, :], True, True, [QT, skt], [sp_])
        cp(SCO.t[:, b4 * 512:(b4 + 1) * 512], sp_.t[:, 0:512], [sp_], [SCO])
        yield
    SV = sm("SV", 256); SI = sm("SI", 256, U32); WK = sm("WK", 128); SIF = sm("SIF", 256)
    SV3 = SV.t[:].rearrange("p (c k) -> p c k", k=16)
    SI3 = SI.t[:].rearrange("p (c k) -> p c k", k=16)
    for c in range(16):
        S.op("dve", lambda e, o=SV3[:, c, 0:8], i=SCO3[:, c, :]: e.max(o, i), [SCO], [SV])
        S.op("dve", lambda e, o=WK.t[:], r=SV3[:, c, 0:8], i=SCO3[:, c, :]: e.match_replace(o, r, i, -1.0e30),
             [SCO, SV], [WK])
        S.op("dve", lambda e, o=SV3[:, c, 8:16], i=WK.t[:]: e.max(o, i), [WK], [SV])
        S.op("dve", lambda e, o=SI3[:, c, 0:8], m=SV3[:, c, 0:8], i=SCO3[:, c, :]: e.max_index(o, m, i),
             [SCO, SV], [SI])
        S.op("dve", lambda e, o=SI3[:, c, 8:16], m=SV3[:, c, 8:16], i=SCO3[:, c, :]: e.max_index(o, m, i),
             [SCO, SV], [SI])
        if c % 4 == 3:
            yield
    cp(SIF.t[:], SI.t[:], [SI], [SIF], eng="dve")
    CAND = Gfull[2]; CW = sm("CW", 256); CS = sm("CS", 128); CI = sm("CI", 128, U32); CIF = sm("CIF", 128)
    SV4 = SV.t[:].rearrange("p (h j k) -> p h j k", j=2, k=16)
    SIF4 = SIF.t[:].rearrange("p (h j k) -> p h j k", j=2, k=16)
    CAND3 = CAND.t[:].rearrange("p (h c) -> p h c", c=256)
    CS3 = CS.t[:].rearrange("p (h k) -> p h k", k=16)
    CI3 = CI.t[:].rearrange("p (h k) -> p h k", k=16)
    for h in range(8):
        tt(CAND3[:, h, :].rearrange("p (a b) -> p a b", b=16),
           SV4[:, h, 0, :].unsqueeze(2).to_broadcast([128, 16, 16]),
           SV4[:, h, 1, :].unsqueeze(1).to_broadcast([128, 16, 16]), ALU.add, [SV], [CAND])
    for h in range(8):
        S.op("dve", lambda e, o=CS3[:, h, 0:8], i=CAND3[:, h, :]: e.max(o, i), [CAND], [CS])
        S.op("dve", lambda e, o=CW.t[:], r=CS3[:, h, 0:8], i=CAND3[:, h, :]: e.match_replace(o, r, i, -1.0e30),
             [CAND, CS], [CW])
        S.op("dve", lambda e, o=CS3[:, h, 8:16], i=CW.t[:]: e.max(o, i), [CW], [CS])
        S.op("dve", lambda e, o=CI3[:, h, 0:8], m=CS3[:, h, 0:8], i=CAND3[:, h, :]: e.max_index(o, m, i),
             [CAND, CS], [CI])
        S.op("dve", lambda e, o=CI3[:, h, 8:16], m=CS3[:, h, 8:16], i=CAND3[:, h, :]: e.max_index(o, m, i),
             [CAND, CS], [CI])
        if h % 4 == 3:
            yield
    CA = sm("CA", 128, U32); CB = sm("CB", 128, U32); CAF = sm("CAF", 128); CBF = sm("CBF", 128)
    ts(CA.t[:], CI.t[:], 4, None, ALU.logical_shift_right, None, [CI], [CA])
    ts(CB.t[:], CI.t[:], 15, None, ALU.bitwise_and, None, [CI], [CB])
    cp(CAF.t[:], CA.t[:], [CA], [CAF], eng="dve")
    cp(CBF.t[:], CB.t[:], [CB], [CBF], eng="dve")
    OH = sm("OH", 1024); I1 = sm("I1", 128); I2 = sm("I2", 128); EIF = sm("EIF", 128); EI = sm("EI", 128, U32)
    for (sel, jj, dst) in ((CAF, 0, I1), (CBF, 1, I2)):
        for half in range(2):
            hs = slice(half * 4, half * 4 + 4)
            OH4 = OH.t[:].rearrange("p (h k a) -> p h k a", k=16, a=16)
            selv = sel.t[:].rearrange("p (h k) -> p h k", k=16)[:, hs, :]
            for hh in range(4):
                h = half * 4 + hh
                tt(OH4[:, hh, :, :], sel.t[:].rearrange("p (h k) -> p h k", k=16)[:, h, :].unsqueeze(2).to_broadcast([128, 16, 16]),
                   iota16.t[:].unsqueeze(1).to_broadcast([128, 16, 16]), ALU.is_equal, [sel, iota16], [OH])
                tt(OH4[:, hh, :, :], OH4[:, hh, :, :],
                   SIF4[:, h, jj, :].unsqueeze(1).to_broadcast([128, 16, 16]), ALU.mult, [OH, SIF], [OH])
            S.op("dve", lambda e, o=dst.t[:, half * 64:(half + 1) * 64], i=OH.t[:].rearrange("p (x a) -> p x a", a=16):
                 e.tensor_reduce(o, i, AX.X, ALU.add), [OH], [dst])
    stt(EIF.t[:], I1.t[:], 128.0, I2.t[:], ALU.mult, ALU.add, [I1, I2], [EIF])
    ts(EIF.t[:], EIF.t[:], 0.0, 16383.0, ALU.max, ALU.min, [EIF], [EIF])
    cp(EI.t[:], EIF.t[:], [EIF], [EI], eng="dve")
    tap("eif", j, EIF, EIF.t[:], 128)
    GW = sm("GW", 128); GS = sm("GS", 8); GR = sm("GR", 8)
    GW3 = GW.t[:].rearrange("p (h k) -> p h k", k=16)
    tt(GW3, CS3, CS3[:, :, 0:1].to_broadcast([128, 8, 16]), ALU.subtract, [CS], [GW])
    act(GW.t[:], GW.t[:], AF.Exp, [GW], [GW])
    S.op("dve", lambda e: e.tensor_reduce(GS.t[:], GW3, AX.X, ALU.add), [GW], [GS])
    S.op("dve", lambda e: e.reciprocal(GR.t[:], GS.t[:]), [GS], [GR])
    tt(GW3, GW3, GR.t[:].unsqueeze(2).to_broadcast([128, 8, 16]), ALU.mult, [GW, GR], [GW])
    tap("gw", j, GW, GW.t[:], 128)
    st.update(EI=EI, GW=GW, U2=U2)
    yield


def peer_back(nc, S, L, st):
    sm = L["sm"]; tt = L["tt"]; stt = L["stt"]; act = L["act"]; memset = L["memset"]; tap = L["tap"]; j = L["j"]
    uv = L["uv"]; FF = L["FF"]
    EI = st["EI"]; GW = st["GW"]; U2 = st["U2"]
    Gh = L["Gh"]; gslot = L["gslot"]; uvb = L["uvb"]; UVB = L["UVB"]
    NS = 6
    ts = L["ts"]; mm = L["mm"]; cp = L["cp"]; FFP = L["FFP"]; identb = L["identb"]; DGr = L["DGr"]
    AP_ = sm("APRE", 128); GEL = sm("GEL", 128); GA = sm("GA", 128); JK = sm("JK", 1024)

    stt = L["stt"]
    FD = FF
    memset(FD.t[:], 0.0, [FD], eng="pool")

    PHS = L["PHS"]
    dec = {}

    def on_dve(s):
        return dec[s]

    def gate(s):
        dec[s] = bool(PHS["dve_acc"]) and s not in (0, 127)
        dg = DGr[s % 3]
        tt(GA.t[:, s:s + 1], GEL.t[:, s:s + 1], GW.t[:, s:s + 1], ALU.mult, [GEL, GW], [GA])
        if not on_dve(s):
            ts(dg.t[:], identb.t[:], GA.t[:, s:s + 1], None, ALU.mult, None, [identb, GA], [dg])

    def accum(s):
        k = s % NS
        if on_dve(s):
            stt(FD.t[:], gslot(k)[:, 1024:2048], GA.t[:, s:s + 1], FD.t[:], ALU.mult, ALU.add, [Gh[k], GA, FD], [FD])
            return
        dg = DGr[s % 3]
        for half in range(2):
            mm(FFP[half].t[:, 0:512], dg.t[:], gslot(k)[:, 1024 + 512 * half:1024 + 512 * (half + 1)],
               s == 0, s == 127, [dg, Gh[k]], [FFP[half]])

    for s in range(128):
        k = s % NS
        gb = Gh[k]
        S.dma("pool", (lambda e, o=gslot(k), idx=EI.t[:, s:s + 1]: e.indirect_dma_start(
            o, None, uvb, bass.IndirectOffsetOnAxis(idx, 0))), gb, reads=[EI, UVB], writes=[gb])
        S.op("dve", lambda e, o=JK.t[:], a=gslot(k)[:, 0:1024], acc=AP_.t[:, s:s + 1]: e.scalar_tensor_tensor(
            o, a, 1.0, U2.t[:], ALU.mult, ALU.mult, accum_out=acc), [gb, U2], [JK, AP_])
        act(GEL.t[:, s:s + 1], AP_.t[:, s:s + 1], AF.Gelu, [AP_], [GEL])
        if s >= 2:
            gate(s - 2)
        if s >= 3:
            accum(s - 3)
        yield
    gate(126); accum(125)
    gate(127); accum(126); accum(127)
    for half in range(2):
        tt(FF.t[:, 512 * half:512 * (half + 1)], FFP[half].t[:, 0:512], FD.t[:, 512 * half:512 * (half + 1)], ALU.add,
           [FFP[half], FD], [FF])
    tap("ff", j, FF, FF.t[:], 1024)


def _blk(W):
    return np.ascontiguousarray(W.reshape(8, 128, 256).transpose(1, 0, 2)).reshape(128, 2048)


def prep_shared(inp):
    f = np.float32
    w_in = np.asarray(inp["w_in"], f)[0]
    blocks = []
    for g in range(8):
        blocks.append(w_in[:, 256 * g:256 * g + 256])
        blocks.append(w_in[:, 2048 + 256 * g:2048 + 256 * g + 256])
        blocks.append(np.concatenate([w_in[:, 4096 + 128 * g:4096 + 128 * g + 128],
                                      w_in[:, 5120 + 128 * g:5120 + 128 * g + 128]], 1))
    o = 6176
    for i in range(4):
        for k in range(3):
            blocks.append(w_in[:, o + 1024 * k + 256 * i:o + 1024 * k + 256 * i + 256])
    og = 9248
    wout = np.asarray(inp["ssd_w_out"], f)[0]
    scw_out = np.asarray(inp["sc_w_out"], f)[0]
    for cb in range(4):
        c0, c1 = 256 * cb, 256 * cb + 256
        blocks.append(w_in[:, og + c0:og + c1])
        blocks.append(w_in[:, og + 1024 + c0:og + 1024 + c1])
        blocks.append(wout[0:1024, c0:c1])
        blocks.append(wout[1024:2048, c0:c1])
        blocks.append(scw_out[:, c0:c1])
    w_o = np.asarray(inp["w_o"], f)[0]
    for cb in range(4):
        blocks.append(w_o[:, 256 * cb:256 * cb + 256])
    wq = np.asarray(inp["peer_w_q"], f)[0]
    for i in range(8):
        blocks.append(wq[:, 256 * i:256 * i + 256])
    assert len(blocks) == NBLK
    wall = np.stack([_blk(b) for b in blocks]).astype(f)
    sh = {"wall": wall}
    sh["uv"] = np.ascontiguousarray(np.concatenate([np.asarray(inp["peer_u"], f)[0],
                                                    np.asarray(inp["peer_v"], f)[0]], axis=1))
    sh["c_ident"] = np.eye(128, dtype=f)
    tri = np.triu(np.ones((128, 128), f))
    sh["c_tri"] = tri
    sh["c_negm"] = ((tri - 1.0) * (-NEG)).astype(f)
    sh["c_ones"] = np.ones((128, 128), f)
    sh["c_iota"] = np.tile(np.arange(16, dtype=f)[None, :], (128, 1))
    row = np.concatenate([np.asarray(inp["ln_ffn"], f)[0], np.asarray(inp["ln_final"], f),
                          np.asarray(inp["ssd_dt_bias"], f)[0], np.asarray(inp["ssd_a_log"], f)[0],
                          np.asarray(inp["ssd_d"], f)[0]])
    sh["c_rows"] = np.ascontiguousarray(np.tile(row[None, :], (128, 1)))
    cwf = np.asarray(inp["ssd_conv_w"], f)[0]
    cbf = np.asarray(inp["ssd_conv_b"], f)[0]
    cw = np.zeros((128, 32, 5), f)
    for g in range(8):
        for i in range(4):
            if i < 2:
                ch = 256 * g + 128 * i
            elif i == 2:
                ch = 2048 + 128 * g
            else:
                ch = 3072 + 128 * g
            cw[:, 4 * g + i, 0:4] = cwf[ch:ch + 128, :]
            cw[:, 4 * g + i, 4] = cbf[ch:ch + 128]
    sh["c_cw"] = cw.reshape(128, 160)
    sh["c_scw"] = np.ascontiguousarray(np.asarray(inp["sc_conv_w"], f)[0].reshape(8, 128, 3).transpose(1, 0, 2)).reshape(128, 24)
    sh["c_wdt"] = np.ascontiguousarray(w_in[:, 6144:6176].reshape(8, 128, 32).transpose(1, 0, 2)).reshape(128, 256)
    sk = np.asarray(inp["peer_sub_keys"], f)[0].reshape(16, 128, 128)
    sh["c_skt"] = np.ascontiguousarray(sk.transpose(2, 0, 1)).reshape(128, 2048)
    sh["c_lnmix"] = np.ascontiguousarray(np.asarray(inp["ln_mix"], f)[0].reshape(8, 128).T)
    sh["c_ngc"] = np.ascontiguousarray(np.asarray(inp["ssd_norm"], f)[0].reshape(16, 128).T)
    return sh


def make_xh(inp, b):
    f = np.float32
    xh = np.zeros((NTOK_EXT, D), f)
    xh[112:128] = np.asarray(inp["meta_tokens"], f)
    xh[128:] = np.asarray(inp["x"], f)[b]
    return xh


_CACHE = {}


def kernel(**inputs):
    if "nc" not in _CACHE:
        _CACHE["nc"] = build()
    nc, _es = _CACHE["nc"]
    sh = prep_shared(inputs)
    in_maps = []
    for b in range(8):
        m = dict(sh)
        m["xh"] = make_xh(inputs, b)
        in_maps.append(m)
    res = run_bass_kernel_spmd(nc, in_maps, core_ids=list(range(8)))
    return np.stack([np.asarray(r["out"], np.float32) for r in res.results], axis=0)
```

```python
import numpy as np
from contextlib import ExitStack
import concourse.bass as bass
import concourse.mybir as mybir
from concourse.bass_utils import run_bass_kernel_spmd

F32 = mybir.dt.float32
F32R = mybir.dt.float32r
BF16 = mybir.dt.bfloat16
U32 = mybir.dt.uint32
ALU = mybir.AluOpType
AF = mybir.ActivationFunctionType
AX = mybir.AxisListType

D = 1024
SEQ = 4096
TT = 128
NT_FULL = SEQ // TT + 1
NTOK_EXT = NT_FULL * TT
EPS = 1e-6
NEG = -1.0e5
NBLK = 68
RW = 3
RG = 5
PIPE_RATIO = 2
NROW = 2048 + 96


class Buf:
    def __init__(self, name, t):
        self.name = name
        self.t = t
        self.w = {}
        self.r = {}
        self.dsem = None
        self.dcnt = 0


class Multi:
    def __init__(self, t, parts):
        self.t = t
        self.parts = parts


class Sched:
    ENG = ("pe", "dve", "act", "pool", "sp")

    def __init__(self, nc, es):
        self.nc = nc
        self.es = es
        self.ops = {e: [] for e in self.ENG}
        self.cnt = {e: 0 for e in self.ENG}
        self.esem = {e: es.enter_context(nc.semaphore("sem_" + e)) for e in ("pe", "dve", "act", "pool")}
        self.waited = {e: {} for e in self.ENG}
        self.final = {}
        self.nins = 0

    def _deps(self, reads, writes):
        deps = {}

        def add(d):
            for k, sv in d.items():
                if k not in deps or deps[k][1] < sv[1]:
                    deps[k] = sv
        for b in reads:
            add(b.w)
        for b in writes:
            add(b.w)
            add(b.r)
        return deps

    def _waits(self, eng, deps, indep=False):
        for k, (s, v) in deps.items():
            if eng == "pe" and k == "pe":
                continue
            if indep and k == eng:
                continue
            if self.waited[eng].get(k, 0) < v:
                self.waited[eng][k] = v
                self.ops[eng].append(("wait", s, v))

    def _update(self, key, ev, reads, writes):
        for b in reads:
            if b in writes:
                continue
            cur = b.r.get(key)
            if cur is None or cur[1] < ev[1]:
                b.r[key] = ev
        for b in writes:
            if b.r:
                b.w = {key: ev}
                b.r = {}
            else:
                cur = b.w.get(key)
                if cur is None or cur[1] < ev[1]:
                    b.w[key] = ev

    @staticmethod
    def _flat(bufs):
        out = []
        for b in bufs:
            if isinstance(b, Multi):
                out.extend(b.parts)
            else:
                out.append(b)
        return out

    def op(self, eng, fn, reads=(), writes=(), indep=False):
        reads = self._flat(reads); writes = self._flat(writes)
        self._waits(eng, self._deps(reads, writes), indep)
        self.cnt[eng] += 1
        ev = (self.esem[eng], self.cnt[eng])
        self.ops[eng].append(("ins", fn, self.esem[eng], 1))
        self._update(eng, ev, reads, writes)
        self.nins += 1

    def dma(self, eng, fn, sembuf, reads=(), writes=(), is_out=False):
        reads = self._flat(reads); writes = self._flat(writes)
        self._waits(eng, self._deps(reads, writes))
        if sembuf.dsem is None:
            sembuf.dsem = self.es.enter_context(self.nc.semaphore("dsem_" + sembuf.name))
        sembuf.dcnt += 16
        key = "d_" + sembuf.name
        ev = (sembuf.dsem, sembuf.dcnt)
        self.ops[eng].append(("ins", fn, sembuf.dsem, 16))
        self._update(key, ev, reads, writes)
        self.final[key] = ev
        self.nins += 1

    def replay(self):
        import os as _os
        for e in ("pe", "dve", "act", "pool"):
            if self.cnt[e] > 0 and _os.environ.get("FINAL_ALL", "1") == "1":
                self.final[e] = (self.esem[e], self.cnt[e])
        for k, (s, v) in self.final.items():
            if self.waited["sp"].get(k, 0) < v:
                self.ops["sp"].append(("wait", s, v))
        ops = self.ops

        allsems = [sv[0] for sv in self.final.values()]

        def mk(e):
            def body(engobj):
                for item in ops[e]:
                    if item[0] == "wait":
                        engobj.wait_ge(item[1], item[2])
                    else:
                        item[1](engobj).then_inc(item[2], item[3])
            return body
        with self.nc.Block() as block:
            block.tensor(mk("pe"))
            block.vector(mk("dve"))
            block.scalar(mk("act"))
            block.gpsimd(mk("pool"))
            block.sync(mk("sp"))


def rr2(gens, width=2, offset=4):
    active = []
    it = iter(gens)
    since = offset
    while True:
        if len(active) < width and (since >= offset or not active):
            g = next(it, None)
            if g is not None:
                active.append(g)
                since = 0
        if not active:
            return
        for g in list(active):
            try:
                next(g)
            except StopIteration:
                active.remove(g)
            else:
                yield
        since += 1


class _Stop(Exception):
    pass


def build(NT=NT_FULL, with_peer=True, taps=(), maxstage=99):
    nc = bass.Bass("TRN2", target_bir_lowering=False)
    es = ExitStack()
    S = Sched(nc, es)

    def stage(n):
        if n > maxstage:
            raise _Stop()

    def dram_in(name, shape, dt=F32):
        return nc.dram_tensor(name, list(shape), dt, kind="ExternalInput").ap()

    xh = dram_in("xh", [NTOK_EXT, D])
    wall = dram_in("wall", [NBLK, 128, 2048])
    uv = dram_in("uv", [16384, 2048])
    d_ident = dram_in("c_ident", [128, 128])
    d_tri = dram_in("c_tri", [128, 128])
    d_negm = dram_in("c_negm", [128, 128])
    d_ones = dram_in("c_ones", [128, 128])
    d_iota = dram_in("c_iota", [128, 16])
    d_rows = dram_in("c_rows", [128, NROW])
    d_cw = dram_in("c_cw", [128, 32 * 5])
    d_scw = dram_in("c_scw", [128, 8 * 3])
    d_wdt = dram_in("c_wdt", [128, 8 * 32])
    d_skt = dram_in("c_skt", [128, 16 * 128])
    d_lnmix = dram_in("c_lnmix", [128, 8])
    d_ngc = dram_in("c_ngc", [128, 16])
    n_out_rows = max((NT - 1) * TT, TT)
    out = nc.dram_tensor("out", [n_out_rows, D], F32, kind="ExternalOutput").ap()
    tap_out = {}
    for (nm, tj, words) in taps:
        tap_out[(nm, tj)] = nc.dram_tensor("tap_%s_%d" % (nm, tj), [128, words], F32, kind="ExternalOutput").ap()

    uvb = nc.dram_tensor("uvb", [16384, 2048], BF16, kind="Internal").ap()
    UVB = Buf("UVB", None)

    def sb(name, words, dt=F32):
        t = es.enter_context(nc.sbuf_tensor(name, [128, words], dt))
        return Buf(name, t)

    def ps(name):
        t = es.enter_context(nc.psum_tensor(name, [128, 512], F32))
        return Buf(name, t)

    ident = sb("ident", 128); tri = sb("tri", 128); negm = sb("negm", 128); ones = sb("ones", 128)
    iota16 = sb("iota16", 16); rows = sb("rows", NROW); cw = sb("cw", 160); scw = sb("scw", 24)
    wdt = sb("wdt", 256); skt = sb("skt", 2048); lnmix = sb("lnmix", 8); ngc = sb("ngc", 16)
    for b_, d_ in ((ident, d_ident), (tri, d_tri), (negm, d_negm), (ones, d_ones), (iota16, d_iota),
                   (rows, d_rows), (cw, d_cw), (scw, d_scw), (wdt, d_wdt), (skt, d_skt),
                   (lnmix, d_lnmix), (ngc, d_ngc)):
        S.dma("sp", (lambda e, o=b_.t[:], i=d_: e.dma_start(o, i)), b_, writes=[b_])
    LNF = rows.t[:, 0:1024]; LNO = rows.t[:, 1024:2048]
    DTB = rows.t[:, 2048:2080]; ALOG = rows.t[:, 2080:2112]; DSK = rows.t[:, 2112:2144]

    P = [ps("ps%d" % i) for i in range(8)]
    pctr = [0]

    FFP = [P[6], P[7]]

    def pnext():
        b = P[pctr[0] % 6]
        pctr[0] += 1
        return b

    identb = Buf("identb", es.enter_context(nc.sbuf_tensor("identb", [128, 128], BF16)))
    DGr = [Buf("DG%d" % i, es.enter_context(nc.sbuf_tensor("DG%d" % i, [128, 128], BF16))) for i in range(3)]

    Hr = [sb("H%d" % i, 1024) for i in range(2)]
    Ub = sb("U", 1024); UT = sb("UT", 1024)
    Wr = [sb("W%d" % i, 2048) for i in range(2)]
    Wc = [sb("Wc%d" % i, 2048, F32R) for i in range(2)]
    XR = [sb("XR%d" % i, 4 * 131) for i in range(2)]
    XC = [sb("XC%d" % i, 512) for i in range(2)]
    HIST = [sb("HIST%d" % g, 12) for g in range(8)]
    ST = [sb("ST%d" % g, 256) for g in range(8)]
    YNT = sb("YNT", 2048)
    Gb = [sb("G%d" % i, 256) for i in range(2)]
    VMG = sb("VMG", 1024)
    Vb = VMG; MG = Ub; MGT = sb("MGT", 1024)
    PH = [sb("PH%d" % c, 130) for c in range(8)]
    SCs = sb("SCs", 256); CACC = sb("CACC", 128)
    GT = []
    for par in range(2):
        GT.append(dict(
            zs=sb("zs%d" % par, 256), XDT=sb("XDT%d" % par, 256), XDW=sb("XDW%d" % par, 256), XSD=sb("XSD%d" % par, 256),
            BTs=sb("BTs%d" % par, 128), CBm=sb("CBm%d" % par, 128), SEG=sb("SEG%d" % par, 512),
            DEC=sb("DEC%d" % par, 512), MT=sb("MT%d" % par, 512), Y1=sb("Y1%d" % par, 256), Y2=sb("Y2%d" % par, 256),
            YZ=sb("YZ%d" % par, 256), YN=sb("YN%d" % par, 256), ACC=sb("ACC%d" % par, 128),
            ADTB=sb("ADTB%d" % par, 512), ss2=sb("ss2%d" % par, 1), rstd2=sb("rstd2%d" % par, 1)))
    TMr = [sb("TM%d" % i, 512) for i in range(2)]
    tmc = [0]

    def tmnext():
        b = TMr[tmc[0] % 2]
        tmc[0] += 1
        return b
    small = {}
    GBt = [es.enter_context(nc.sbuf_tensor("GBt%d" % i, [128, 2048], F32)) for i in range(3)]
    Gh = [Buf("Gh%d" % k, GBt[k // 2]) for k in range(6)]
    Gfull = [Multi(GBt[i], [Gh[2 * i], Gh[2 * i + 1]]) for i in range(3)]

    def gslot(k):
        return GBt[k // 2][:].bitcast(BF16)[:, (k % 2) * 2048:(k % 2 + 1) * 2048]

    def sm(name, words=32, dt=F32):
        if name not in small:
            small[name] = sb(name, words, dt)
        return small[name]

    aneg = sm("aneg"); ss = sm("ss", 1); rstd = sm("rstd", 1)

    def tt(out_, in0, in1, op, r, w, eng="dve", indep=False):
        S.op(eng, lambda e: e.tensor_tensor(out_, in0, in1, op), r, w, indep=indep)

    def ts(out_, in0, s1, s2, op0, op1, r, w, eng="dve"):
        if s2 is None:
            S.op(eng, lambda e: e.tensor_scalar(out_, in0, s1, None, op0), r, w)
        else:
            S.op(eng, lambda e: e.tensor_scalar(out_, in0, s1, s2, op0, op1), r, w)

    def stt(out_, in0, scalar, in1, op0, op1, r, w):
        S.op("dve", lambda e: e.scalar_tensor_tensor(out_, in0, scalar, in1, op0, op1), r, w)

    zero = sb("zero", 1)

    def act(out_, in_, func, r, w, **kw):
        indep = kw.pop("indep", False)
        if "bias" not in kw:
            kw["bias"] = zero.t[:, 0:1]
            r = list(r) + [zero]
        S.op("act", lambda e: e.activation(out_, in_, func, **kw), r, w, indep=indep)

    def cp(out_, in_, r, w, eng="act"):
        if eng == "act":
            S.op("act", lambda e: e.copy(out_, in_), r, w)
        else:
            S.op(eng, lambda e: e.tensor_copy(out_, in_), r, w)

    def mm(out_, lhsT, rhs, start, stop, r, w):
        S.op("pe", lambda e: e.matmul(out_, lhsT, rhs, start=start, stop=stop), r, w)

    def mmr(out_, lhsT, rhs, start, stop, r, w):
        S.op("pe", lambda e: e.matmul(out_, lhsT.bitcast(F32R), rhs.bitcast(F32R), start=start, stop=stop), r, w)

    def tr(out_, in_, r, w):
        S.op("pe", lambda e: e.transpose(out_, in_, ident.t[:]), list(r) + [ident], w)

    def memset(ap, val, w, eng="dve"):
        S.op(eng, lambda e: e.memset(ap, val), (), w)

    def tap(name, j, buf, ap, words):
        if (name, j) in tap_out:
            S.dma("sp", (lambda e, o=tap_out[(name, j)], i=ap: e.dma_start(o, i)), buf, reads=[buf], is_out=True)

    req = []
    wpos = [0]
    wloaded = [0]
    mq = {"m": 0, "q": 0}

    def wload():
        p = wloaded[0]
        slot = Wr[p % 2]
        S.dma("sp", (lambda e, o=slot.t[:], p=p: e.dma_start(o, wall[req[p] if p < len(req) else 0])), slot,
              writes=[slot])
        wloaded[0] += 1

    def wget(exact=False):
        if exact:
            req.append(60 + mq["q"] % 8)
            mq["q"] += 1
        else:
            req.append(mq["m"] % 60)
            mq["m"] += 1
        st = Wr[wpos[0] % 2]
        if exact:
            return st, st.t[:].rearrange("p (k c) -> p k c", c=256)
        wc = Wc[wpos[0] % 2]
        cp(wc.t[:], st.t[:], [st], [wc])
        return wc, wc.t[:].rearrange("p (k c) -> p k c", c=256)

    def wdone():
        wpos[0] += 1
        wload()

    for _ in range(2):
        wload()

    def init_ops():
        memset(zero.t[:], 0.0, [zero])
        cp(identb.t[:], ident.t[:], [ident], [identb], eng="dve")
        stage(0.2)
        act(aneg.t[:, 0:32], ALOG, AF.Exp, [rows], [aneg])
        ts(aneg.t[:, 0:32], aneg.t[:, 0:32], -1.0, None, ALU.mult, None, [aneg], [aneg])
        stage(0.3)
        for g in range(8):
            memset(HIST[g].t[:], 0.0, [HIST[g]], eng="pool")
            memset(ST[g].t[:], 0.0, [ST[g]], eng="pool")
        for c in range(8):
            memset(PH[c].t[:], 0.0, [PH[c]], eng="pool")
        stage(0.4)


    def rmsnorm_stats(src, junk, ssb, rsb):
        act(junk.t[:, 0:1024], src.t[:, 0:1024], AF.Square, [src], [junk, ssb], accum_out=ssb.t[:, 0:1])
        ts(ssb.t[:, 0:1], ssb.t[:, 0:1], 1.0 / 1024.0, EPS, ALU.mult, ALU.add, [ssb], [ssb])
        act(ssb.t[:, 0:1], ssb.t[:, 0:1], AF.Sqrt, [ssb], [ssb])
        S.op("dve", lambda e: e.reciprocal(rsb.t[:, 0:1], ssb.t[:, 0:1]), [ssb], [rsb])

    def transpose8(src, dst, scale_col=None, r32=False):
        for half in range(2):
            pb = pnext()
            for q in range(4):
                kt = half * 4 + q
                tr(pb.t[:, q * 128:(q + 1) * 128], src.t[:, kt * 128:(kt + 1) * 128], [src], [pb])
            o = dst.t[:, half * 512:(half + 1) * 512]
            if r32:
                o = o.bitcast(F32R)
            if scale_col is None:
                cp(o, pb.t[:, 0:512], [pb], [dst], eng="dve")
            else:
                sc_b, sc_ap = scale_col
                tt(o.rearrange("p (k t) -> p k t", t=128), pb.t[:, 0:512].rearrange("p (k t) -> p k t", t=128),
                   sc_ap[:, half * 4:(half + 1) * 4].unsqueeze(2).to_broadcast([128, 4, 128]), ALU.mult,
                   [pb, sc_b], [dst])

    UT3 = UT.t[:].rearrange("p (k t) -> p k t", t=128)
    YNT3 = YNT.t[:].rearrange("p (k t) -> p k t", t=128)
    MGT3 = MGT.t[:].rearrange("p (k t) -> p k t", t=128)
    wdt3 = wdt.t[:].rearrange("p (k c) -> p k c", c=32)
    cw3 = cw.t[:].rearrange("p (c k) -> p c k", k=5)
    scw3 = scw.t[:].rearrange("p (c k) -> p c k", k=3)

    ENV = dict(sm=sm, tt=tt, ts=ts, stt=stt, act=act, cp=cp, mm=mm, pnext=pnext, wget=wget, wdone=wdone,
               memset=memset, tap=tap, rows=rows, LNF=LNF, skt=skt, iota16=iota16, uv=uv,
               transpose8=transpose8, rmsnorm_stats=rmsnorm_stats, YNT=YNT, VMG=VMG, MGT=MGT,
               Gh=Gh, Gfull=Gfull, gslot=gslot, uvb=uvb, UVB=UVB, FFP=FFP, identb=identb, DGr=DGr)

    def mixer(j):
        H = Hr[j % 2]
        S.dma("sp", (lambda e, o=H.t[:], i=xh[j * TT:(j + 1) * TT, :]: e.dma_start(o, i)), H, writes=[H])
        stage(0.5)
        rmsnorm_stats(H, Ub, ss, rstd)
        stage(0.6)
        ts(Ub.t[:], H.t[:], rstd.t[:, 0:1], None, ALU.mult, None, [H, rstd], [Ub])
        tap("U", j, Ub, Ub.t[:], 1024)
        stage(0.7)
        transpose8(Ub, UT, scale_col=(lnmix, lnmix.t), r32=True)
        tap("UT", j, UT, UT.t[:], 1024)
        stage(1)
        yield

        dtp = pnext()
        for kt in range(8):
            mm(dtp.t[:, 0:32], UT3[:, kt, :], wdt3[:, kt, :], kt == 0, kt == 7, [UT, wdt], [dtp])
        xd = sm("xd"); ax = sm("ax"); ex = sm("ex"); dtb = sm("dt"); adt = sm("adt")
        acum = sm("acum"); Eb = sm("E"); wb = sm("w"); eA = sm("eA"); dif = sm("dif")
        tt(xd.t[:], dtp.t[:, 0:32], DTB, ALU.add, [dtp, rows], [xd])
        stage(1.2)
        stt(ax.t[:], xd.t[:], -1.0, xd.t[:], ALU.mult, ALU.max, [xd], [ax])
        act(ex.t[:], ax.t[:], AF.Exp, [ax], [ex], scale=-1.0)
        stage(1.4)
        ts(ex.t[:], ex.t[:], 1.0, None, ALU.add, None, [ex], [ex])
        act(ex.t[:], ex.t[:], AF.Ln, [ex], [ex])
        stage(1.5)
        stt(dtb.t[:], xd.t[:], 0.0, ex.t[:], ALU.max, ALU.add, [xd, ex], [dtb])
        tt(adt.t[:], dtb.t[:], aneg.t[:], ALU.mult, [dtb, aneg], [adt])
        tap("dt", j, dtb, dtb.t[:], 32)
        stage(1.6)
        cp_ = pnext()
        mm(cp_.t[:, 0:32], tri.t[:], adt.t[:], True, True, [tri, adt], [cp_])
        mm(cp_.t[:, 32:64], ones.t[:], adt.t[:], True, True, [ones, adt], [cp_])
        stage(1.7)
        cp(acum.t[:], cp_.t[:, 0:32], [cp_], [acum], eng="dve")
        act(Eb.t[:], cp_.t[:, 0:32], AF.Exp, [cp_], [Eb])
        act(eA.t[:], cp_.t[:, 32:64], AF.Exp, [cp_], [eA])
        stage(1.8)
        tt(dif.t[:], cp_.t[:, 32:64], acum.t[:], ALU.subtract, [cp_, acum], [dif])
        act(wb.t[:], dif.t[:], AF.Exp, [dif], [wb])
        stage(2)
        yield

        def group(g):
            T = GT[g % 2]
            zs = T['zs']; XDT = T['XDT']; XDW = T['XDW']; XSD = T['XSD']; BTs = T['BTs']; CBm = T['CBm']
            SEG = T['SEG']; DEC = T['DEC']; MT = T['MT']; Y1 = T['Y1']; Y2 = T['Y2']; YZ = T['YZ']; YN = T['YN']
            ACC = T['ACC']; ADTB = T['ADTB']; ss2 = T['ss2']; rstd2 = T['rstd2']
            xr = XR[g % 2]; xc = XC[g % 2]
            xr3 = xr.t[:].rearrange("p (c t) -> p c t", t=131)
            xc3 = xc.t[:].rearrange("p (c t) -> p c t", t=128)
            wbuf, w3 = wget()
            zp = pnext()
            for kt in range(8):
                mmr(zp.t[:, 0:256], UT3[:, kt, :], w3[:, kt, :], kt == 0, kt == 7, [UT, wbuf], [zp])
            wdone()
            stage(2.05)
            act(zs.t[:], zp.t[:, 0:256], AF.Silu, [zp], [zs])
            stage(2.1)
            tmq = pnext()
            for half in range(2):
                wbuf, w3 = wget()
                for kt in range(8):
                    mmr(tmq.t[:, half * 256:(half + 1) * 256], UT3[:, kt, :], w3[:, kt, :], kt == 0, kt == 7,
                        [UT, wbuf], [tmq])
                wdone()
            TM = tmnext()
            cp(TM.t[:], tmq.t[:, 0:512], [tmq], [TM])
            xp = pnext()
            for i in range(4):
                tr(xp.t[:, i * 128:(i + 1) * 128], TM.t[:, i * 128:(i + 1) * 128], [TM], [xp])
            stage(2.2)
            cp(xr3[:, :, 0:3], HIST[g].t[:].rearrange("p (c t) -> p c t", t=3), [HIST[g]], [xr], eng="pool")
            stage(2.25)
            cp(xr3[:, :, 3:131], xp.t[:, 0:512].rearrange("p (c t) -> p c t", t=128), [xp], [xr])
            cp(HIST[g].t[:].rearrange("p (c t) -> p c t", t=3), xr3[:, :, 128:131], [xr], [HIST[g]], eng="pool")
            stage(2.3)
            for i in range(4):
                c = 4 * g + i
                ts(ACC.t[:], xr3[:, i, 0:128], cw3[:, c, 0:1], None, ALU.mult, None, [xr, cw], [ACC])
                for k in range(1, 4):
                    stt(ACC.t[:], xr3[:, i, k:k + 128], cw3[:, c, k:k + 1], ACC.t[:], ALU.mult, ALU.add,
                        [xr, cw, ACC], [ACC])
                stage(2.4)
                act(xc3[:, i, :], ACC.t[:], AF.Silu, [ACC, cw], [xc], bias=cw3[:, c, 4:5], indep=True)
                stage(2.5)
            if j == 0:
                memset(xc3[:, :, 0:112], 0.0, [xc])
            if g == 0:
                tap("xc", j, xc, xc.t[:], 512)
            stage(3)
            yield
            tp = pnext()
            for i in range(3):
                tr(tp.t[:, i * 128:(i + 1) * 128], xc3[:, i, :], [xc], [tp])
            stage(3.1)
            hs = slice(4 * g, 4 * g + 4)
            tp3 = tp.t[:, 0:256].rearrange("p (r q) -> p r q", q=64)
            tt(XDT.t[:].rearrange("p (r q) -> p r q", q=64), tp3,
               dtb.t[:, hs].unsqueeze(2).to_broadcast([128, 4, 64]), ALU.mult, [tp, dtb], [XDT])
            tt(XSD.t[:].rearrange("p (r q) -> p r q", q=64), tp3,
               DSK[:, hs].unsqueeze(2).to_broadcast([128, 4, 64]), ALU.mult, [tp, rows], [XSD])
            stage(3.2)
            cp(BTs.t[:], tp.t[:, 256:384], [tp], [BTs], eng="dve")
            stage(3.25)
            tt(XDW.t[:].rearrange("p (r q) -> p r q", q=64), XDT.t[:].rearrange("p (r q) -> p r q", q=64),
               wb.t[:, hs].unsqueeze(2).to_broadcast([128, 4, 64]), ALU.mult, [XDT, wb], [XDW])
            stage(3.3)
            yield
            cbp = pnext()
            mm(cbp.t[:, 0:128], xc3[:, 2, :], xc3[:, 3, :], True, True, [xc], [cbp])
            tt(CBm.t[:], cbp.t[:, 0:128], tri.t[:], ALU.mult, [cbp, tri], [CBm])
            stage(3.4)
            yield
            rp = pnext()
            cp(ADTB.t[:].rearrange("p (r m) -> p r m", m=128),
               adt.t[:, 4 * g:4 * g + 4].unsqueeze(2).to_broadcast([128, 4, 128]), [adt], [ADTB], eng="dve")
            for r in range(4):
                h = 4 * g + r
                mm(rp.t[:, r * 128:(r + 1) * 128], ADTB.t[:, r * 128:(r + 1) * 128], tri.t[:],
                   True, True, [ADTB, tri], [rp])
            stage(3.5)
            for r in range(4):
                h = 4 * g + r
                stt(SEG.t[:, r * 128:(r + 1) * 128], rp.t[:, r * 128:(r + 1) * 128], acum.t[:, h:h + 1], negm.t[:],
                    ALU.subtract, ALU.add, [rp, acum, negm], [SEG])
            act(DEC.t[:], SEG.t[:], AF.Exp, [SEG], [DEC])
            stage(3.7)
            tt(MT.t[:].rearrange("p (r l) -> p r l", l=128), DEC.t[:].rearrange("p (r l) -> p r l", l=128),
               CBm.t[:].unsqueeze(1).to_broadcast([128, 4, 128]), ALU.mult, [DEC, CBm], [MT])
            stage(4)
            yield
            yp = pnext()
            for r in range(4):
                mm(yp.t[:, r * 64:(r + 1) * 64], MT.t[:, r * 128:(r + 1) * 128], XDT.t[:, r * 64:(r + 1) * 64],
                   True, True, [MT, XDT], [yp])
            mm(yp.t[:, 256:512], xc3[:, 3, :], ST[g].t[:], True, True, [xc, ST[g]], [yp])
            tt(Y1.t[:], yp.t[:, 0:256], XSD.t[:], ALU.add, [yp, XSD], [Y1])
            tt(Y2.t[:].rearrange("p (r q) -> p r q", q=64), yp.t[:, 256:512].rearrange("p (r q) -> p r q", q=64),
               Eb.t[:, hs].unsqueeze(2).to_broadcast([128, 4, 64]), ALU.mult, [yp, Eb], [Y2])
            tt(Y1.t[:], Y1.t[:], Y2.t[:], ALU.add, [Y1, Y2], [Y1])
            yield
            sp_ = pnext()
            mm(sp_.t[:, 0:256], BTs.t[:], XDW.t[:], True, True, [BTs, XDW], [sp_])
            tt(ST[g].t[:].rearrange("p (r q) -> p r q", q=64), ST[g].t[:].rearrange("p (r q) -> p r q", q=64),
               eA.t[:, hs].unsqueeze(2).to_broadcast([128, 4, 64]), ALU.mult, [ST[g], eA], [ST[g]])
            tt(ST[g].t[:], ST[g].t[:], sp_.t[:, 0:256], ALU.add, [ST[g], sp_], [ST[g]])
            yield
            tt(YZ.t[:], Y1.t[:], zs.t[:], ALU.mult, [Y1, zs], [YZ])
            act(YN.t[:], YZ.t[:], AF.Square, [YZ], [YN, ss2], accum_out=ss2.t[:, 0:1])
            ts(ss2.t[:, 0:1], ss2.t[:, 0:1], 1.0 / 256.0, EPS, ALU.mult, ALU.add, [ss2], [ss2])
            act(ss2.t[:, 0:1], ss2.t[:, 0:1], AF.Sqrt, [ss2], [ss2])
            S.op("dve", lambda e: e.reciprocal(rstd2.t[:, 0:1], ss2.t[:, 0:1]), [ss2], [rstd2])
            ts(YN.t[:], YZ.t[:], rstd2.t[:, 0:1], None, ALU.mult, None, [YZ, rstd2], [YN])
            if g == 0:
                tap("yn", j, YN, YN.t[:], 256)
            stage(5)
            yield
            np_ = pnext()
            for i in range(2):
                tr(np_.t[:, i * 128:(i + 1) * 128], YN.t[:, i * 128:(i + 1) * 128], [YN], [np_])
            tt(YNT3[:, 2 * g:2 * g + 2, :].bitcast(F32R), np_.t[:, 0:256].rearrange("p (k t) -> p k t", t=128),
               ngc.t[:, 2 * g:2 * g + 2].unsqueeze(2).to_broadcast([128, 2, 128]), ALU.mult, [np_, ngc], [YNT])
            yield

        yield from rr2([group(g) for g in range(8)])

        V3 = Vb.t[:, 0:1024].rearrange("p (c t) -> p c t", t=128)
        for i in range(4):
            sbp = pnext(); scp = pnext(); sxp = pnext()
            for pb in (sbp, scp, sxp):
                tmq = pnext()
                wbuf, w3 = wget()
                for kt in range(8):
                    mmr(tmq.t[:, 0:256], UT3[:, kt, :], w3[:, kt, :], kt == 0, kt == 7, [UT, wbuf], [tmq])
                wdone()
                TM = tmnext()
                cp(TM.t[:, 0:256], tmq.t[:, 0:256], [tmq], [TM])
                for q in range(2):
                    tr(pb.t[:, q * 128:(q + 1) * 128], TM.t[:, q * 128:(q + 1) * 128], [TM], [pb])
            cp(SCs.t[:], scp.t[:, 0:256], [scp], [SCs])
            for q in range(2):
                c = 2 * i + q
                ph = PH[c]
                tt(ph.t[:, 2:130], sxp.t[:, q * 128:(q + 1) * 128], SCs.t[:, q * 128:(q + 1) * 128], ALU.mult,
                   [sxp, SCs], [ph])
                ts(CACC.t[:], ph.t[:, 0:128], scw3[:, c, 0:1], None, ALU.mult, None, [ph, scw], [CACC])
                for k in range(1, 3):
                    stt(CACC.t[:], ph.t[:, k:k + 128], scw3[:, c, k:k + 1], CACC.t[:], ALU.mult, ALU.add,
                        [ph, scw, CACC], [CACC])
                tt(V3[:, c, :].bitcast(F32R), sbp.t[:, q * 128:(q + 1) * 128], CACC.t[:], ALU.mult, [sbp, CACC], [Vb])
                cp(ph.t[:, 0:2], ph.t[:, 128:130], [ph], [ph], eng="pool")
            yield

        stage(6)
        for cb in range(4):
            cs_ = slice(cb * 256, (cb + 1) * 256)
            for gi in range(2):
                wbuf, w3 = wget()
                gp = pnext()
                for kt in range(8):
                    mmr(gp.t[:, 0:256], UT3[:, kt, :], w3[:, kt, :], kt == 0, kt == 7, [UT, wbuf], [gp])
                wdone()
                act(Gb[gi].t[:, 0:256], gp.t[:, 0:256], AF.Sigmoid, [gp], [Gb[gi]])
            yssd = pnext()
            for kh in range(2):
                wbuf, w3 = wget()
                for kt in range(8):
                    mmr(yssd.t[:, 0:256], YNT3[:, kh * 8 + kt, :], w3[:, kt, :], kh == 0 and kt == 0,
                       kh == 1 and kt == 7, [YNT, wbuf], [yssd])
                wdone()
            tt(MG.t[:, cs_], yssd.t[:, 0:256], Gb[0].t[:, 0:256], ALU.mult, [yssd, Gb[0]], [MG])
            ysc = pnext()
            wbuf, w3 = wget()
            for kt in range(8):
                mmr(ysc.t[:, 0:256], V3[:, kt, :], w3[:, kt, :], kt == 0, kt == 7, [Vb, wbuf], [ysc])
            wdone()
            tt(Gb[1].t[:, 0:256], ysc.t[:, 0:256], Gb[1].t[:, 0:256], ALU.mult, [ysc, Gb[1]], [Gb[1]])
            tt(MG.t[:, cs_], MG.t[:, cs_], Gb[1].t[:, 0:256], ALU.add, [MG, Gb[1]], [MG])
            yield
        transpose8(MG, MGT, r32=True)
        for cb in range(4):
            cs_ = slice(cb * 256, (cb + 1) * 256)
            wbuf, w3 = wget()
            op_ = pnext()
            for kt in range(8):
                mmr(op_.t[:, 0:256], MGT3[:, kt, :], w3[:, kt, :], kt == 0, kt == 7, [MGT, wbuf], [op_])
            wdone()
            tt(H.t[:, cs_], H.t[:, cs_], op_.t[:, 0:256], ALU.add, [H, op_], [H])
            yield
        tap("h2", j, H, H.t[:], 1024)


    def back(j, H, st):
        FF = sm("FF", 1024)
        if with_peer:
            yield from peer_back(nc, S, dict(ENV, j=j, H=H, FF=FF), st)
        else:
            memset(FF.t[:], 0.0, [FF], eng="pool")
        tt(FF.t[:], FF.t[:], H.t[:], ALU.add, [FF, H], [FF])
        ss3 = sm("ss3", 1); rstd3 = sm("rstd3", 1); junk3 = sm("JK", 1024)
        rmsnorm_stats(FF, junk3, ss3, rstd3)
        stt(FF.t[:], FF.t[:], rstd3.t[:, 0:1], LNO, ALU.mult, ALU.mult, [FF, rstd3, rows], [FF])
        S.dma("sp", (lambda e, o=out[(j - 1) * TT:j * TT, :], i=FF.t[:]: e.dma_start(o, i)), FF, reads=[FF],
              is_out=True)
        yield

    def tail(j):
        H = Hr[j % 2]
        st = {}
        if with_peer:
            yield from peer_front(nc, S, dict(ENV, j=j, H=H, FF=sm("FF", 1024)), st)
        yield from back(j, H, st)

    def convert_uv():
        outs = [sm("JK", 1024), sm("FF", 1024)]
        cvs = [Buf("cvs%d" % i, None) for i in range(3)]
        for c in range(128):
            stg = Gfull[c % 3]
            S.dma("sp", (lambda e, o=stg.t[:], i=uv[c * 128:(c + 1) * 128, :]: e.dma_start(o, i)), cvs[c % 3],
                  writes=[stg])
            ob = outs[c % 2]
            cp(ob.t[:].bitcast(BF16), stg.t[:], [stg], [ob], eng="pool")
            S.dma("sp", (lambda e, o=uvb[c * 128:(c + 1) * 128, :], i=ob.t[:].bitcast(BF16): e.dma_start(o, i)), ob,
                  reads=[ob], writes=[UVB])
            yield

    def drain(gen):
        if gen is not None:
            for _ in gen:
                pass

    def interleave(gm, gb, ratio):
        for _ in gm:
            if gb is not None:
                for _k in range(ratio):
                    try:
                        next(gb)
                    except StopIteration:
                        gb = None
                        break
        drain(gb)

    try:
        init_ops()
        pending = convert_uv() if (with_peer and NT > 1) else None
        for j in range(NT):
            interleave(mixer(j), pending, PIPE_RATIO)
            pending = tail(j) if j >= 1 else None
        drain(pending)
    except _Stop:
        pass
    S.replay()
    es.close()
    return nc, es


def peer_front(nc, S, L, st):
    sm = L["sm"]; tt = L["tt"]; ts = L["ts"]; stt = L["stt"]; act = L["act"]; cp = L["cp"]; mm = L["mm"]
    pnext = L["pnext"]; wget = L["wget"]; wdone = L["wdone"]; memset = L["memset"]; tap = L["tap"]; j = L["j"]
    H = L["H"]; rows = L["rows"]; LNF = L["LNF"]; skt = L["skt"]; iota16 = L["iota16"]; uv = L["uv"]
    transpose8 = L["transpose8"]; rmsnorm_stats = L["rmsnorm_stats"]
    Gfull = L["Gfull"]
    QT = Gfull[0]; SCO = Gfull[1]; U2T = sm("U2T", 1024); FF = L["FF"]
    U2 = sm("U2", 1024); ssp = sm("ssp", 1); rsp = sm("rsp", 1)
    rmsnorm_stats(H, FF, ssp, rsp)
    stt(U2.t[:], H.t[:], rsp.t[:, 0:1], LNF, ALU.mult, ALU.mult, [H, rsp, rows], [U2])
    transpose8(U2, U2T)
    U2T3 = U2T.t[:].rearrange("p (k t) -> p k t", t=128)
    QT3 = QT.t[:].rearrange("p (c t) -> p c t", t=128)
    skt3 = skt.t[:].rearrange("p (c k) -> p c k", k=128)
    for i in range(8):
        wbuf, w3 = wget(exact=True)
        qp = pnext()
        for q in range(2):
            for kt in range(8):
                mm(qp.t[:, q * 128:(q + 1) * 128], w3[:, kt, q * 128:(q + 1) * 128], U2T3[:, kt, :],
                   kt == 0, kt == 7, [U2T, wbuf], [qp])
        wdone()
        cp(QT.t[:, i * 256:(i + 1) * 256], qp.t[:, 0:256], [qp], [QT])
        yield
    SCO3 = SCO.t[:].rearrange("p (c k) -> p c k", k=128)
    for b4 in range(4):
        sp_ = pnext()
        for q in range(4):
            c = b4 * 4 + q
            mm(sp_.t[:, q * 128:(q + 1) * 128], QT3[:, c, :], skt3[:, c, :], True, True, [QT, skt], [sp_])
        cp(SCO.t[:, b4 * 512:(b4 + 1) * 512], sp_.t[:, 0:512], [sp_], [SCO])
        yield
    SV = sm("SV", 256); SI = sm("SI", 256, U32); WK = sm("WK", 128); SIF = sm("SIF", 256)
    SV3 = SV.t[:].rearrange("p (c k) -> p c k", k=16)
    SI3 = SI.t[:].rearrange("p (c k) -> p c k", k=16)
    for c in range(16):
        S.op("dve", lambda e, o=SV3[:, c, 0:8], i=SCO3[:, c, :]: e.max(o, i), [SCO], [SV])
        S.op("dve", lambda e, o=WK.t[:], r=SV3[:, c, 0:8], i=SCO3[:, c, :]: e.match_replace(o, r, i, -1.0e30),
             [SCO, SV], [WK])
        S.op("dve", lambda e, o=SV3[:, c, 8:16], i=WK.t[:]: e.max(o, i), [WK], [SV])
        S.op("dve", lambda e, o=SI3[:, c, 0:8], m=SV3[:, c, 0:8], i=SCO3[:, c, :]: e.max_index(o, m, i),
             [SCO, SV], [SI])
        S.op("dve", lambda e, o=SI3[:, c, 8:16], m=SV3[:, c, 8:16], i=SCO3[:, c, :]: e.max_index(o, m, i),
             [SCO, SV], [SI])
        if c % 4 == 3:
            yield
    cp(SIF.t[:], SI.t[:], [SI], [SIF], eng="dve")
    CAND = Gfull[2]; CW = sm("CW", 256); CS = sm("CS", 128); CI = sm("CI", 128, U32); CIF = sm("CIF", 128)
    SV4 = SV.t[:].rearrange("p (h j k) -> p h j k", j=2, k=16)
    SIF4 = SIF.t[:].rearrange("p (h j k) -> p h j k", j=2, k=16)
    CAND3 = CAND.t[:].rearrange("p (h c) -> p h c", c=256)
    CS3 = CS.t[:].rearrange("p (h k) -> p h k", k=16)
    CI3 = CI.t[:].rearrange("p (h k) -> p h k", k=16)
    for h in range(8):
        tt(CAND3[:, h, :].rearrange("p (a b) -> p a b", b=16),
           SV4[:, h, 0, :].unsqueeze(2).to_broadcast([128, 16, 16]),
           SV4[:, h, 1, :].unsqueeze(1).to_broadcast([128, 16, 16]), ALU.add, [SV], [CAND])
    for h in range(8):
        S.op("dve", lambda e, o=CS3[:, h, 0:8], i=CAND3[:, h, :]: e.max(o, i), [CAND], [CS])
        S.op("dve", lambda e, o=CW.t[:], r=CS3[:, h, 0:8], i=CAND3[:, h, :]: e.match_replace(o, r, i, -1.0e30),
             [CAND, CS], [CW])
        S.op("dve", lambda e, o=CS3[:, h, 8:16], i=CW.t[:]: e.max(o, i), [CW], [CS])
        S.op("dve", lambda e, o=CI3[:, h, 0:8], m=CS3[:, h, 0:8], i=CAND3[:, h, :]: e.max_index(o, m, i),
             [CAND, CS], [CI])
        S.op("dve", lambda e, o=CI3[:, h, 8:16], m=CS3[:, h, 8:16], i=CAND3[:, h, :]: e.max_index(o, m, i),
             [CAND, CS], [CI])
        if h % 4 == 3:
            yield
    CA = sm("CA", 128, U32); CB = sm("CB", 128, U32); CAF = sm("CAF", 128); CBF = sm("CBF", 128)
    ts(CA.t[:], CI.t[:], 4, None, ALU.logical_shift_right, None, [CI], [CA])
    ts(CB.t[:], CI.t[:], 15, None, ALU.bitwise_and, None, [CI], [CB])
    cp(CAF.t[:], CA.t[:], [CA], [CAF], eng="dve")
    cp(CBF.t[:], CB.t[:], [CB], [CBF], eng="dve")
    OH = sm("OH", 1024); I1 = sm("I1", 128); I2 = sm("I2", 128); EIF = sm("EIF", 128); EI = sm("EI", 128, U32)
    for (sel, jj, dst) in ((CAF, 0, I1), (CBF, 1, I2)):
        for half in range(2):
            hs = slice(half * 4, half * 4 + 4)
            OH4 = OH.t[:].rearrange("p (h k a) -> p h k a", k=16, a=16)
            selv = sel.t[:].rearrange("p (h k) -> p h k", k=16)[:, hs, :]
            for hh in range(4):
                h = half * 4 + hh
                tt(OH4[:, hh, :, :], sel.t[:].rearrange("p (h k) -> p h k", k=16)[:, h, :].unsqueeze(2).to_broadcast([128, 16, 16]),
                   iota16.t[:].unsqueeze(1).to_broadcast([128, 16, 16]), ALU.is_equal, [sel, iota16], [OH])
                tt(OH4[:, hh, :, :], OH4[:, hh, :, :],
                   SIF4[:, h, jj, :].unsqueeze(1).to_broadcast([128, 16, 16]), ALU.mult, [OH, SIF], [OH])
            S.op("dve", lambda e, o=dst.t[:, half * 64:(half + 1) * 64], i=OH.t[:].rearrange("p (x a) -> p x a", a=16):
                 e.tensor_reduce(o, i, AX.X, ALU.add), [OH], [dst])
    stt(EIF.t[:], I1.t[:], 128.0, I2.t[:], ALU.mult, ALU.add, [I1, I2], [EIF])
    ts(EIF.t[:], EIF.t[:], 0.0, 16383.0, ALU.max, ALU.min, [EIF], [EIF])
    cp(EI.t[:], EIF.t[:], [EIF], [EI], eng="dve")
    tap("eif", j, EIF, EIF.t[:], 128)
    GW = sm("GW", 128); GS = sm("GS", 8); GR = sm("GR", 8)
    GW3 = GW.t[:].rearrange("p (h k) -> p h k", k=16)
    tt(GW3, CS3, CS3[:, :, 0:1].to_broadcast([128, 8, 16]), ALU.subtract, [CS], [GW])
    act(GW.t[:], GW.t[:], AF.Exp, [GW], [GW])
    S.op("dve", lambda e: e.tensor_reduce(GS.t[:], GW3, AX.X, ALU.add), [GW], [GS])
    S.op("dve", lambda e: e.reciprocal(GR.t[:], GS.t[:]), [GS], [GR])
    tt(GW3, GW3, GR.t[:].unsqueeze(2).to_broadcast([128, 8, 16]), ALU.mult, [GW, GR], [GW])
    tap("gw", j, GW, GW.t[:], 128)
    st.update(EI=EI, GW=GW, U2=U2)
    yield


def peer_back(nc, S, L, st):
    sm = L["sm"]; tt = L["tt"]; stt = L["stt"]; act = L["act"]; memset = L["memset"]; tap = L["tap"]; j = L["j"]
    uv = L["uv"]; FF = L["FF"]
    EI = st["EI"]; GW = st["GW"]; U2 = st["U2"]
    Gh = L["Gh"]; gslot = L["gslot"]; uvb = L["uvb"]; UVB = L["UVB"]
    NS = 6
    ts = L["ts"]; mm = L["mm"]; cp = L["cp"]; FFP = L["FFP"]; identb = L["identb"]; DGr = L["DGr"]
    AP_ = sm("APRE", 128); GEL = sm("GEL", 128); GA = sm("GA", 128); JK = sm("JK", 1024)

    def gate(s):
        dg = DGr[s % 3]
        tt(GA.t[:, s:s + 1], GEL.t[:, s:s + 1], GW.t[:, s:s + 1], ALU.mult, [GEL, GW], [GA], indep=True)
        ts(dg.t[:], identb.t[:], GA.t[:, s:s + 1], None, ALU.mult, None, [identb, GA], [dg])

    def accum(s):
        k = s % NS
        dg = DGr[s % 3]
        for half in range(2):
            mm(FFP[half].t[:, 0:512], dg.t[:], gslot(k)[:, 1024 + 512 * half:1024 + 512 * (half + 1)],
               s == 0, s == 127, [dg, Gh[k]], [FFP[half]])

    for s in range(128):
        k = s % NS
        gb = Gh[k]
        S.dma("pool", (lambda e, o=gslot(k), idx=EI.t[:, s:s + 1]: e.indirect_dma_start(
            o, None, uvb, bass.IndirectOffsetOnAxis(idx, 0))), gb, reads=[EI, UVB], writes=[gb])
        S.op("dve", lambda e, o=JK.t[:], a=gslot(k)[:, 0:1024], acc=AP_.t[:, s:s + 1]: e.scalar_tensor_tensor(
            o, a, 1.0, U2.t[:], ALU.mult, ALU.mult, accum_out=acc), [gb, U2], [JK, AP_], indep=True)
        act(GEL.t[:, s:s + 1], AP_.t[:, s:s + 1], AF.Gelu, [AP_], [GEL], indep=True)
        if s >= 2:
            gate(s - 2)
        if s >= 3:
            accum(s - 3)
        yield
    gate(126); accum(125)
    gate(127); accum(126); accum(127)
    for half in range(2):
        cp(FF.t[:, 512 * half:512 * (half + 1)], FFP[half].t[:, 0:512], [FFP[half]], [FF], eng="dve")
    tap("ff", j, FF, FF.t[:], 1024)


def _blk(W):
    return np.ascontiguousarray(W.reshape(8, 128, 256).transpose(1, 0, 2)).reshape(128, 2048)


def prep_shared(inp):
    f = np.float32
    w_in = np.asarray(inp["w_in"], f)[0]
    blocks = []
    for g in range(8):
        blocks.append(w_in[:, 256 * g:256 * g + 256])
        blocks.append(w_in[:, 2048 + 256 * g:2048 + 256 * g + 256])
        blocks.append(np.concatenate([w_in[:, 4096 + 128 * g:4096 + 128 * g + 128],
                                      w_in[:, 5120 + 128 * g:5120 + 128 * g + 128]], 1))
    o = 6176
    for i in range(4):
        for k in range(3):
            blocks.append(w_in[:, o + 1024 * k + 256 * i:o + 1024 * k + 256 * i + 256])
    og = 9248
    wout = np.asarray(inp["ssd_w_out"], f)[0]
    scw_out = np.asarray(inp["sc_w_out"], f)[0]
    for cb in range(4):
        c0, c1 = 256 * cb, 256 * cb + 256
        blocks.append(w_in[:, og + c0:og + c1])
        blocks.append(w_in[:, og + 1024 + c0:og + 1024 + c1])
        blocks.append(wout[0:1024, c0:c1])
        blocks.append(wout[1024:2048, c0:c1])
        blocks.append(scw_out[:, c0:c1])
    w_o = np.asarray(inp["w_o"], f)[0]
    for cb in range(4):
        blocks.append(w_o[:, 256 * cb:256 * cb + 256])
    wq = np.asarray(inp["peer_w_q"], f)[0]
    for i in range(8):
        blocks.append(wq[:, 256 * i:256 * i + 256])
    assert len(blocks) == NBLK
    wall = np.stack([_blk(b) for b in blocks]).astype(f)
    sh = {"wall": wall}
    sh["uv"] = np.ascontiguousarray(np.concatenate([np.asarray(inp["peer_u"], f)[0],
                                                    np.asarray(inp["peer_v"], f)[0]], axis=1))
    sh["c_ident"] = np.eye(128, dtype=f)
    tri = np.triu(np.ones((128, 128), f))
    sh["c_tri"] = tri
    sh["c_negm"] = ((tri - 1.0) * (-NEG)).astype(f)
    sh["c_ones"] = np.ones((128, 128), f)
    sh["c_iota"] = np.tile(np.arange(16, dtype=f)[None, :], (128, 1))
    row = np.concatenate([np.asarray(inp["ln_ffn"], f)[0], np.asarray(inp["ln_final"], f),
                          np.asarray(inp["ssd_dt_bias"], f)[0], np.asarray(inp["ssd_a_log"], f)[0],
                          np.asarray(inp["ssd_d"], f)[0]])
    sh["c_rows"] = np.ascontiguousarray(np.tile(row[None, :], (128, 1)))
    cwf = np.asarray(inp["ssd_conv_w"], f)[0]
    cbf = np.asarray(inp["ssd_conv_b"], f)[0]
    cw = np.zeros((128, 32, 5), f)
    for g in range(8):
        for i in range(4):
            if i < 2:
                ch = 256 * g + 128 * i
            elif i == 2:
                ch = 2048 + 128 * g
            else:
                ch = 3072 + 128 * g
            cw[:, 4 * g + i, 0:4] = cwf[ch:ch + 128, :]
            cw[:, 4 * g + i, 4] = cbf[ch:ch + 128]
    sh["c_cw"] = cw.reshape(128, 160)
    sh["c_scw"] = np.ascontiguousarray(np.asarray(inp["sc_conv_w"], f)[0].reshape(8, 128, 3).transpose(1, 0, 2)).reshape(128, 24)
    sh["c_wdt"] = np.ascontiguousarray(w_in[:, 6144:6176].reshape(8, 128, 32).transpose(1, 0, 2)).reshape(128, 256)
    sk = np.asarray(inp["peer_sub_keys"], f)[0].reshape(16, 128, 128)
    sh["c_skt"] = np.ascontiguousarray(sk.transpose(2, 0, 1)).reshape(128, 2048)
    sh["c_lnmix"] = np.ascontiguousarray(np.asarray(inp["ln_mix"], f)[0].reshape(8, 128).T)
    sh["c_ngc"] = np.ascontiguousarray(np.asarray(inp["ssd_norm"], f)[0].reshape(16, 128).T)
    return sh


def make_xh(inp, b):
    f = np.float32
    xh = np.zeros((NTOK_EXT, D), f)
    xh[112:128] = np.asarray(inp["meta_tokens"], f)
    xh[128:] = np.asarray(inp["x"], f)[b]
    return xh


_CACHE = {}


def kernel(**inputs):
    if "nc" not in _CACHE:
        _CACHE["nc"] = build()
    nc, _es = _CACHE["nc"]
    sh = prep_shared(inputs)
    in_maps = []
    for b in range(8):
        m = dict(sh)
        m["xh"] = make_xh(inputs, b)
        in_maps.append(m)
    res = run_bass_kernel_spmd(nc, in_maps, core_ids=list(range(8)))
    return np.stack([np.asarray(r["out"], np.float32) for r in res.results], axis=0)
```

```python
import numpy as np
from contextlib import ExitStack
import concourse.bass as bass
import concourse.mybir as mybir
from concourse.bass_utils import run_bass_kernel_spmd

F32 = mybir.dt.float32
F32R = mybir.dt.float32r
BF16 = mybir.dt.bfloat16
U32 = mybir.dt.uint32
ALU = mybir.AluOpType
AF = mybir.ActivationFunctionType
AX = mybir.AxisListType

D = 1024
SEQ = 4096
TT = 128
NT_FULL = SEQ // TT + 1
NTOK_EXT = NT_FULL * TT
EPS = 1e-6
NEG = -1.0e5
NBLK = 68
RW = 3
RG = 5
PIPE_RATIO = 2
NROW = 2048 + 96


class Buf:
    def __init__(self, name, t):
        self.name = name
        self.t = t
        self.w = {}
        self.r = {}
        self.dsem = None
        self.dcnt = 0


class Multi:
    def __init__(self, t, parts):
        self.t = t
        self.parts = parts


class Sched:
    ENG = ("pe", "dve", "act", "pool", "sp")

    def __init__(self, nc, es):
        self.nc = nc
        self.es = es
        self.ops = {e: [] for e in self.ENG}
        self.cnt = {e: 0 for e in self.ENG}
        self.esem = {e: es.enter_context(nc.semaphore("sem_" + e)) for e in ("pe", "dve", "act", "pool")}
        self.waited = {e: {} for e in self.ENG}
        self.final = {}
        self.nins = 0

    def _deps(self, reads, writes):
        deps = {}

        def add(d):
            for k, sv in d.items():
                if k not in deps or deps[k][1] < sv[1]:
                    deps[k] = sv
        for b in reads:
            add(b.w)
        for b in writes:
            add(b.w)
            add(b.r)
        return deps

    def _waits(self, eng, deps, indep=False):
        for k, (s, v) in deps.items():
            if eng == "pe" and k == "pe":
                continue
            if indep and k == eng:
                continue
            if self.waited[eng].get(k, 0) < v:
                self.waited[eng][k] = v
                self.ops[eng].append(("wait", s, v))

    def _update(self, key, ev, reads, writes):
        for b in reads:
            if b in writes:
                continue
            cur = b.r.get(key)
            if cur is None or cur[1] < ev[1]:
                b.r[key] = ev
        for b in writes:
            if b.r:
                b.w = {key: ev}
                b.r = {}
            else:
                cur = b.w.get(key)
                if cur is None or cur[1] < ev[1]:
                    b.w[key] = ev

    @staticmethod
    def _flat(bufs):
        out = []
        for b in bufs:
            if isinstance(b, Multi):
                out.extend(b.parts)
            else:
                out.append(b)
        return out

    def op(self, eng, fn, reads=(), writes=(), indep=False):
        reads = self._flat(reads); writes = self._flat(writes)
        self._waits(eng, self._deps(reads, writes), indep)
        self.cnt[eng] += 1
        ev = (self.esem[eng], self.cnt[eng])
        self.ops[eng].append(("ins", fn, self.esem[eng], 1))
        self._update(eng, ev, reads, writes)
        self.nins += 1

    def dma(self, eng, fn, sembuf, reads=(), writes=(), is_out=False):
        reads = self._flat(reads); writes = self._flat(writes)
        self._waits(eng, self._deps(reads, writes))
        if sembuf.dsem is None:
            sembuf.dsem = self.es.enter_context(self.nc.semaphore("dsem_" + sembuf.name))
        sembuf.dcnt += 16
        key = "d_" + sembuf.name
        ev = (sembuf.dsem, sembuf.dcnt)
        self.ops[eng].append(("ins", fn, sembuf.dsem, 16))
        self._update(key, ev, reads, writes)
        self.final[key] = ev
        self.nins += 1

    def replay(self):
        import os as _os
        for e in ("pe", "dve", "act", "pool"):
            if self.cnt[e] > 0 and _os.environ.get("FINAL_ALL", "1") == "1":
                self.final[e] = (self.esem[e], self.cnt[e])
        for k, (s, v) in self.final.items():
            if self.waited["sp"].get(k, 0) < v:
                self.ops["sp"].append(("wait", s, v))
        ops = self.ops

        allsems = [sv[0] for sv in self.final.values()]

        def mk(e):
            def body(engobj):
                for item in ops[e]:
                    if item[0] == "wait":
                        engobj.wait_ge(item[1], item[2])
                    else:
                        item[1](engobj).then_inc(item[2], item[3])
            return body
        with self.nc.Block() as block:
            block.tensor(mk("pe"))
            block.vector(mk("dve"))
            block.scalar(mk("act"))
            block.gpsimd(mk("pool"))
            block.sync(mk("sp"))


def rr2(gens, width=2, offset=4):
    active = []
    it = iter(gens)
    since = offset
    while True:
        if len(active) < width and (since >= offset or not active):
            g = next(it, None)
            if g is not None:
                active.append(g)
                since = 0
        if not active:
            return
        for g in list(active):
            try:
                next(g)
            except StopIteration:
                active.remove(g)
            else:
                yield
        since += 1


class _Stop(Exception):
    pass


def build(NT=NT_FULL, with_peer=True, taps=(), maxstage=99):
    nc = bass.Bass("TRN2", target_bir_lowering=False)
    es = ExitStack()
    S = Sched(nc, es)

    def stage(n):
        if n > maxstage:
            raise _Stop()

    def dram_in(name, shape, dt=F32):
        return nc.dram_tensor(name, list(shape), dt, kind="ExternalInput").ap()

    xh = dram_in("xh", [NTOK_EXT, D])
    wall = dram_in("wall", [NBLK, 128, 2048])
    uv = dram_in("uv", [16384, 2048])
    d_ident = dram_in("c_ident", [128, 128])
    d_tri = dram_in("c_tri", [128, 128])
    d_negm = dram_in("c_negm", [128, 128])
    d_ones = dram_in("c_ones", [128, 128])
    d_iota = dram_in("c_iota", [128, 16])
    d_rows = dram_in("c_rows", [128, NROW])
    d_cw = dram_in("c_cw", [128, 32 * 5])
    d_scw = dram_in("c_scw", [128, 8 * 3])
    d_wdt = dram_in("c_wdt", [128, 8 * 32])
    d_skt = dram_in("c_skt", [128, 16 * 128])
    d_lnmix = dram_in("c_lnmix", [128, 8])
    d_ngc = dram_in("c_ngc", [128, 16])
    n_out_rows = max((NT - 1) * TT, TT)
    out = nc.dram_tensor("out", [n_out_rows, D], F32, kind="ExternalOutput").ap()
    tap_out = {}
    for (nm, tj, words) in taps:
        tap_out[(nm, tj)] = nc.dram_tensor("tap_%s_%d" % (nm, tj), [128, words], F32, kind="ExternalOutput").ap()

    uvb = nc.dram_tensor("uvb", [16384, 2048], BF16, kind="Internal").ap()
    UVB = Buf("UVB", None)

    def sb(name, words, dt=F32):
        t = es.enter_context(nc.sbuf_tensor(name, [128, words], dt))
        return Buf(name, t)

    def ps(name):
        t = es.enter_context(nc.psum_tensor(name, [128, 512], F32))
        return Buf(name, t)

    ident = sb("ident", 128); tri = sb("tri", 128); negm = sb("negm", 128); ones = sb("ones", 128)
    iota16 = sb("iota16", 16); rows = sb("rows", NROW); cw = sb("cw", 160); scw = sb("scw", 24)
    wdt = sb("wdt", 256); skt = sb("skt", 2048); lnmix = sb("lnmix", 8); ngc = sb("ngc", 16)
    for b_, d_ in ((ident, d_ident), (tri, d_tri), (negm, d_negm), (ones, d_ones), (iota16, d_iota),
                   (rows, d_rows), (cw, d_cw), (scw, d_scw), (wdt, d_wdt), (skt, d_skt),
                   (lnmix, d_lnmix), (ngc, d_ngc)):
        S.dma("sp", (lambda e, o=b_.t[:], i=d_: e.dma_start(o, i)), b_, writes=[b_])
    LNF = rows.t[:, 0:1024]; LNO = rows.t[:, 1024:2048]
    DTB = rows.t[:, 2048:2080]; ALOG = rows.t[:, 2080:2112]; DSK = rows.t[:, 2112:2144]

    P = [ps("ps%d" % i) for i in range(8)]
    pctr = [0]

    FFP = [P[6], P[7]]

    def pnext():
        b = P[pctr[0] % 6]
        pctr[0] += 1
        return b

    identb = Buf("identb", es.enter_context(nc.sbuf_tensor("identb", [128, 128], BF16)))
    DGr = [Buf("DG%d" % i, es.enter_context(nc.sbuf_tensor("DG%d" % i, [128, 128], BF16))) for i in range(3)]

    Hr = [sb("H%d" % i, 1024) for i in range(2)]
    Ub = sb("U", 1024); UT = sb("UT", 1024)
    Wr = [sb("W%d" % i, 2048) for i in range(2)]
    Wc = [sb("Wc%d" % i, 2048, F32R) for i in range(2)]
    XR = [sb("XR%d" % i, 4 * 131) for i in range(2)]
    XC = [sb("XC%d" % i, 512) for i in range(2)]
    HIST = [sb("HIST%d" % g, 12) for g in range(8)]
    ST = [sb("ST%d" % g, 256) for g in range(8)]
    YNT = sb("YNT", 2048)
    Gb = [sb("G%d" % i, 256) for i in range(2)]
    VMG = sb("VMG", 1024)
    Vb = VMG; MG = Ub; MGT = sb("MGT", 1024)
    PH = [sb("PH%d" % c, 130) for c in range(8)]
    SCs = sb("SCs", 256); CACC = sb("CACC", 128)
    GT = []
    for par in range(2):
        GT.append(dict(
            zs=sb("zs%d" % par, 256), XDT=sb("XDT%d" % par, 256), XDW=sb("XDW%d" % par, 256), XSD=sb("XSD%d" % par, 256),
            BTs=sb("BTs%d" % par, 128), CBm=sb("CBm%d" % par, 128), SEG=sb("SEG%d" % par, 512),
            DEC=sb("DEC%d" % par, 512), MT=sb("MT%d" % par, 512), Y1=sb("Y1%d" % par, 256), Y2=sb("Y2%d" % par, 256),
            YZ=sb("YZ%d" % par, 256), YN=sb("YN%d" % par, 256), ACC=sb("ACC%d" % par, 128), ACCb=sb("ACCb%d" % par, 128),
            ADTB=sb("ADTB%d" % par, 512), ss2=sb("ss2%d" % par, 1), rstd2=sb("rstd2%d" % par, 1)))
    TMr = [sb("TM%d" % i, 512) for i in range(2)]
    tmc = [0]

    def tmnext():
        b = TMr[tmc[0] % 2]
        tmc[0] += 1
        return b
    small = {}
    GBt = [es.enter_context(nc.sbuf_tensor("GBt%d" % i, [128, 2048], F32)) for i in range(3)]
    Gh = [Buf("Gh%d" % k, GBt[k // 2]) for k in range(6)]
    Gfull = [Multi(GBt[i], [Gh[2 * i], Gh[2 * i + 1]]) for i in range(3)]

    def gslot(k):
        return GBt[k // 2][:].bitcast(BF16)[:, (k % 2) * 2048:(k % 2 + 1) * 2048]

    def sm(name, words=32, dt=F32):
        if name not in small:
            small[name] = sb(name, words, dt)
        return small[name]

    aneg = sm("aneg"); ss = sm("ss", 1); rstd = sm("rstd", 1)

    def tt(out_, in0, in1, op, r, w, eng="dve", indep=False):
        S.op(eng, lambda e: e.tensor_tensor(out_, in0, in1, op), r, w, indep=indep)

    def ts(out_, in0, s1, s2, op0, op1, r, w, eng="dve"):
        if s2 is None:
            S.op(eng, lambda e: e.tensor_scalar(out_, in0, s1, None, op0), r, w)
        else:
            S.op(eng, lambda e: e.tensor_scalar(out_, in0, s1, s2, op0, op1), r, w)

    def stt(out_, in0, scalar, in1, op0, op1, r, w):
        S.op("dve", lambda e: e.scalar_tensor_tensor(out_, in0, scalar, in1, op0, op1), r, w)

    zero = sb("zero", 1)

    def act(out_, in_, func, r, w, **kw):
        indep = kw.pop("indep", False)
        if "bias" not in kw:
            kw["bias"] = zero.t[:, 0:1]
            r = list(r) + [zero]
        S.op("act", lambda e: e.activation(out_, in_, func, **kw), r, w, indep=indep)

    def cp(out_, in_, r, w, eng="act"):
        if eng == "act":
            S.op("act", lambda e: e.copy(out_, in_), r, w)
        else:
            S.op(eng, lambda e: e.tensor_copy(out_, in_), r, w)

    def mm(out_, lhsT, rhs, start, stop, r, w):
        S.op("pe", lambda e: e.matmul(out_, lhsT, rhs, start=start, stop=stop), r, w)

    def mmr(out_, lhsT, rhs, start, stop, r, w):
        S.op("pe", lambda e: e.matmul(out_, lhsT.bitcast(F32R), rhs.bitcast(F32R), start=start, stop=stop), r, w)

    def tr(out_, in_, r, w):
        S.op("pe", lambda e: e.transpose(out_, in_, ident.t[:]), list(r) + [ident], w)

    def memset(ap, val, w, eng="dve"):
        S.op(eng, lambda e: e.memset(ap, val), (), w)

    def tap(name, j, buf, ap, words):
        if (name, j) in tap_out:
            S.dma("sp", (lambda e, o=tap_out[(name, j)], i=ap: e.dma_start(o, i)), buf, reads=[buf], is_out=True)

    req = []
    wpos = [0]
    wloaded = [0]
    mq = {"m": 0, "q": 0}

    def wload():
        p = wloaded[0]
        slot = Wr[p % 2]
        S.dma("sp", (lambda e, o=slot.t[:], p=p: e.dma_start(o, wall[req[p] if p < len(req) else 0])), slot,
              writes=[slot])
        wloaded[0] += 1

    def wget(exact=False):
        if exact:
            req.append(60 + mq["q"] % 8)
            mq["q"] += 1
        else:
            req.append(mq["m"] % 60)
            mq["m"] += 1
        st = Wr[wpos[0] % 2]
        if exact:
            return st, st.t[:].rearrange("p (k c) -> p k c", c=256)
        wc = Wc[wpos[0] % 2]
        cp(wc.t[:], st.t[:], [st], [wc])
        return wc, wc.t[:].rearrange("p (k c) -> p k c", c=256)

    def wdone():
        wpos[0] += 1
        wload()

    for _ in range(2):
        wload()

    def init_ops():
        memset(zero.t[:], 0.0, [zero])
        cp(identb.t[:], ident.t[:], [ident], [identb], eng="dve")
        stage(0.2)
        act(aneg.t[:, 0:32], ALOG, AF.Exp, [rows], [aneg])
        ts(aneg.t[:, 0:32], aneg.t[:, 0:32], -1.0, None, ALU.mult, None, [aneg], [aneg])
        stage(0.3)
        for g in range(8):
            memset(HIST[g].t[:], 0.0, [HIST[g]], eng="pool")
            memset(ST[g].t[:], 0.0, [ST[g]], eng="pool")
        for c in range(8):
            memset(PH[c].t[:], 0.0, [PH[c]], eng="pool")
        stage(0.4)


    def rmsnorm_stats(src, junk, ssb, rsb):
        act(junk.t[:, 0:1024], src.t[:, 0:1024], AF.Square, [src], [junk, ssb], accum_out=ssb.t[:, 0:1])
        ts(ssb.t[:, 0:1], ssb.t[:, 0:1], 1.0 / 1024.0, EPS, ALU.mult, ALU.add, [ssb], [ssb])
        act(ssb.t[:, 0:1], ssb.t[:, 0:1], AF.Sqrt, [ssb], [ssb])
        S.op("dve", lambda e: e.reciprocal(rsb.t[:, 0:1], ssb.t[:, 0:1]), [ssb], [rsb])

    def transpose8(src, dst, scale_col=None, r32=False):
        for half in range(2):
            pb = pnext()
            for q in range(4):
                kt = half * 4 + q
                tr(pb.t[:, q * 128:(q + 1) * 128], src.t[:, kt * 128:(kt + 1) * 128], [src], [pb])
            o = dst.t[:, half * 512:(half + 1) * 512]
            if r32:
                o = o.bitcast(F32R)
            if scale_col is None:
                cp(o, pb.t[:, 0:512], [pb], [dst], eng="dve")
            else:
                sc_b, sc_ap = scale_col
                tt(o.rearrange("p (k t) -> p k t", t=128), pb.t[:, 0:512].rearrange("p (k t) -> p k t", t=128),
                   sc_ap[:, half * 4:(half + 1) * 4].unsqueeze(2).to_broadcast([128, 4, 128]), ALU.mult,
                   [pb, sc_b], [dst])

    UT3 = UT.t[:].rearrange("p (k t) -> p k t", t=128)
    YNT3 = YNT.t[:].rearrange("p (k t) -> p k t", t=128)
    MGT3 = MGT.t[:].rearrange("p (k t) -> p k t", t=128)
    wdt3 = wdt.t[:].rearrange("p (k c) -> p k c", c=32)
    cw3 = cw.t[:].rearrange("p (c k) -> p c k", k=5)
    scw3 = scw.t[:].rearrange("p (c k) -> p c k", k=3)

    ENV = dict(sm=sm, tt=tt, ts=ts, stt=stt, act=act, cp=cp, mm=mm, pnext=pnext, wget=wget, wdone=wdone,
               memset=memset, tap=tap, rows=rows, LNF=LNF, skt=skt, iota16=iota16, uv=uv,
               transpose8=transpose8, rmsnorm_stats=rmsnorm_stats, YNT=YNT, VMG=VMG, MGT=MGT,
               Gh=Gh, Gfull=Gfull, gslot=gslot, uvb=uvb, UVB=UVB, FFP=FFP, identb=identb, DGr=DGr)

    def mixer(j):
        H = Hr[j % 2]
        S.dma("sp", (lambda e, o=H.t[:], i=xh[j * TT:(j + 1) * TT, :]: e.dma_start(o, i)), H, writes=[H])
        stage(0.5)
        rmsnorm_stats(H, Ub, ss, rstd)
        stage(0.6)
        ts(Ub.t[:], H.t[:], rstd.t[:, 0:1], None, ALU.mult, None, [H, rstd], [Ub])
        tap("U", j, Ub, Ub.t[:], 1024)
        stage(0.7)
        transpose8(Ub, UT, scale_col=(lnmix, lnmix.t), r32=True)
        tap("UT", j, UT, UT.t[:], 1024)
        stage(1)
        yield

        dtp = pnext()
        for kt in range(8):
            mm(dtp.t[:, 0:32], UT3[:, kt, :], wdt3[:, kt, :], kt == 0, kt == 7, [UT, wdt], [dtp])
        xd = sm("xd"); ax = sm("ax"); ex = sm("ex"); dtb = sm("dt"); adt = sm("adt")
        acum = sm("acum"); Eb = sm("E"); wb = sm("w"); eA = sm("eA"); dif = sm("dif")
        tt(xd.t[:], dtp.t[:, 0:32], DTB, ALU.add, [dtp, rows], [xd])
        stage(1.2)
        stt(ax.t[:], xd.t[:], -1.0, xd.t[:], ALU.mult, ALU.max, [xd], [ax])
        act(ex.t[:], ax.t[:], AF.Exp, [ax], [ex], scale=-1.0)
        stage(1.4)
        ts(ex.t[:], ex.t[:], 1.0, None, ALU.add, None, [ex], [ex])
        act(ex.t[:], ex.t[:], AF.Ln, [ex], [ex])
        stage(1.5)
        stt(dtb.t[:], xd.t[:], 0.0, ex.t[:], ALU.max, ALU.add, [xd, ex], [dtb])
        tt(adt.t[:], dtb.t[:], aneg.t[:], ALU.mult, [dtb, aneg], [adt])
        tap("dt", j, dtb, dtb.t[:], 32)
        stage(1.6)
        cp_ = pnext()
        mm(cp_.t[:, 0:32], tri.t[:], adt.t[:], True, True, [tri, adt], [cp_])
        mm(cp_.t[:, 32:64], ones.t[:], adt.t[:], True, True, [ones, adt], [cp_])
        stage(1.7)
        cp(acum.t[:], cp_.t[:, 0:32], [cp_], [acum], eng="dve")
        act(Eb.t[:], cp_.t[:, 0:32], AF.Exp, [cp_], [Eb])
        act(eA.t[:], cp_.t[:, 32:64], AF.Exp, [cp_], [eA])
        stage(1.8)
        tt(dif.t[:], cp_.t[:, 32:64], acum.t[:], ALU.subtract, [cp_, acum], [dif])
        act(wb.t[:], dif.t[:], AF.Exp, [dif], [wb])
        stage(2)
        yield

        def group(g):
            T = GT[g % 2]
            zs = T['zs']; XDT = T['XDT']; XDW = T['XDW']; XSD = T['XSD']; BTs = T['BTs']; CBm = T['CBm']
            SEG = T['SEG']; DEC = T['DEC']; MT = T['MT']; Y1 = T['Y1']; Y2 = T['Y2']; YZ = T['YZ']; YN = T['YN']
            ACC2 = [T['ACC'], T['ACCb']]; ADTB = T['ADTB']; ss2 = T['ss2']; rstd2 = T['rstd2']
            xr = XR[g % 2]; xc = XC[g % 2]
            xr3 = xr.t[:].rearrange("p (c t) -> p c t", t=131)
            xc3 = xc.t[:].rearrange("p (c t) -> p c t", t=128)
            wbuf, w3 = wget()
            zp = pnext()
            for kt in range(8):
                mmr(zp.t[:, 0:256], UT3[:, kt, :], w3[:, kt, :], kt == 0, kt == 7, [UT, wbuf], [zp])
            wdone()
            stage(2.05)
            act(zs.t[:], zp.t[:, 0:256], AF.Silu, [zp], [zs])
            stage(2.1)
            tmq = pnext()
            for half in range(2):
                wbuf, w3 = wget()
                for kt in range(8):
                    mmr(tmq.t[:, half * 256:(half + 1) * 256], UT3[:, kt, :], w3[:, kt, :], kt == 0, kt == 7,
                        [UT, wbuf], [tmq])
                wdone()
            TM = tmnext()
            cp(TM.t[:], tmq.t[:, 0:512], [tmq], [TM])
            xp = pnext()
            for i in range(4):
                tr(xp.t[:, i * 128:(i + 1) * 128], TM.t[:, i * 128:(i + 1) * 128], [TM], [xp])
            stage(2.2)
            cp(xr3[:, :, 0:3], HIST[g].t[:].rearrange("p (c t) -> p c t", t=3), [HIST[g]], [xr], eng="pool")
            stage(2.25)
            cp(xr3[:, :, 3:131], xp.t[:, 0:512].rearrange("p (c t) -> p c t", t=128), [xp], [xr])
            cp(HIST[g].t[:].rearrange("p (c t) -> p c t", t=3), xr3[:, :, 128:131], [xr], [HIST[g]], eng="pool")
            stage(2.3)
            for i in range(4):
                c = 4 * g + i
                ACC = ACC2[i % 2]
                ts(ACC.t[:], xr3[:, i, 0:128], cw3[:, c, 0:1], None, ALU.mult, None, [xr, cw], [ACC])
                for k in range(1, 4):
                    stt(ACC.t[:], xr3[:, i, k:k + 128], cw3[:, c, k:k + 1], ACC.t[:], ALU.mult, ALU.add,
                        [xr, cw, ACC], [ACC])
                stage(2.4)
                act(xc3[:, i, :], ACC.t[:], AF.Silu, [ACC, cw], [xc], bias=cw3[:, c, 4:5], indep=True)
                stage(2.5)
            if j == 0:
                memset(xc3[:, :, 0:112], 0.0, [xc])
            if g == 0:
                tap("xc", j, xc, xc.t[:], 512)
            stage(3)
            yield
            tp = pnext()
            for i in range(3):
                tr(tp.t[:, i * 128:(i + 1) * 128], xc3[:, i, :], [xc], [tp])
            stage(3.1)
            hs = slice(4 * g, 4 * g + 4)
            tp3 = tp.t[:, 0:256].rearrange("p (r q) -> p r q", q=64)
            tt(XDT.t[:].rearrange("p (r q) -> p r q", q=64), tp3,
               dtb.t[:, hs].unsqueeze(2).to_broadcast([128, 4, 64]), ALU.mult, [tp, dtb], [XDT])
            tt(XSD.t[:].rearrange("p (r q) -> p r q", q=64), tp3,
               DSK[:, hs].unsqueeze(2).to_broadcast([128, 4, 64]), ALU.mult, [tp, rows], [XSD])
            stage(3.2)
            cp(BTs.t[:], tp.t[:, 256:384], [tp], [BTs], eng="dve")
            stage(3.25)
            tt(XDW.t[:].rearrange("p (r q) -> p r q", q=64), XDT.t[:].rearrange("p (r q) -> p r q", q=64),
               wb.t[:, hs].unsqueeze(2).to_broadcast([128, 4, 64]), ALU.mult, [XDT, wb], [XDW])
            stage(3.3)
            yield
            cbp = pnext()
            mm(cbp.t[:, 0:128], xc3[:, 2, :], xc3[:, 3, :], True, True, [xc], [cbp])
            tt(CBm.t[:], cbp.t[:, 0:128], tri.t[:], ALU.mult, [cbp, tri], [CBm])
            stage(3.4)
            yield
            rp = pnext()
            cp(ADTB.t[:].rearrange("p (r m) -> p r m", m=128),
               adt.t[:, 4 * g:4 * g + 4].unsqueeze(2).to_broadcast([128, 4, 128]), [adt], [ADTB], eng="dve")
            for r in range(4):
                h = 4 * g + r
                mm(rp.t[:, r * 128:(r + 1) * 128], ADTB.t[:, r * 128:(r + 1) * 128], tri.t[:],
                   True, True, [ADTB, tri], [rp])
            stage(3.5)
            for r in range(4):
                h = 4 * g + r
                S.op("dve", lambda e, o=SEG.t[:, r * 128:(r + 1) * 128], i0=rp.t[:, r * 128:(r + 1) * 128],
                     sc_=acum.t[:, h:h + 1]: e.scalar_tensor_tensor(o, i0, sc_, negm.t[:], ALU.subtract, ALU.add),
                     [rp, acum, negm], [SEG], indep=(r > 0))
            act(DEC.t[:], SEG.t[:], AF.Exp, [SEG], [DEC])
            stage(3.7)
            tt(MT.t[:].rearrange("p (r l) -> p r l", l=128), DEC.t[:].rearrange("p (r l) -> p r l", l=128),
               CBm.t[:].unsqueeze(1).to_broadcast([128, 4, 128]), ALU.mult, [DEC, CBm], [MT])
            stage(4)
            yield
            yp = pnext()
            for r in range(4):
                mm(yp.t[:, r * 64:(r + 1) * 64], MT.t[:, r * 128:(r + 1) * 128], XDT.t[:, r * 64:(r + 1) * 64],
                   True, True, [MT, XDT], [yp])
            mm(yp.t[:, 256:512], xc3[:, 3, :], ST[g].t[:], True, True, [xc, ST[g]], [yp])
            tt(Y1.t[:], yp.t[:, 0:256], XSD.t[:], ALU.add, [yp, XSD], [Y1])
            tt(Y2.t[:].rearrange("p (r q) -> p r q", q=64), yp.t[:, 256:512].rearrange("p (r q) -> p r q", q=64),
               Eb.t[:, hs].unsqueeze(2).to_broadcast([128, 4, 64]), ALU.mult, [yp, Eb], [Y2])
            tt(Y1.t[:], Y1.t[:], Y2.t[:], ALU.add, [Y1, Y2], [Y1])
            yield
            sp_ = pnext()
            mm(sp_.t[:, 0:256], BTs.t[:], XDW.t[:], True, True, [BTs, XDW], [sp_])
            tt(ST[g].t[:].rearrange("p (r q) -> p r q", q=64), ST[g].t[:].rearrange("p (r q) -> p r q", q=64),
               eA.t[:, hs].unsqueeze(2).to_broadcast([128, 4, 64]), ALU.mult, [ST[g], eA], [ST[g]])
            tt(ST[g].t[:], ST[g].t[:], sp_.t[:, 0:256], ALU.add, [ST[g], sp_], [ST[g]])
            yield
            tt(YZ.t[:], Y1.t[:], zs.t[:], ALU.mult, [Y1, zs], [YZ])
            act(YN.t[:], YZ.t[:], AF.Square, [YZ], [YN, ss2], accum_out=ss2.t[:, 0:1])
            ts(ss2.t[:, 0:1], ss2.t[:, 0:1], 1.0 / 256.0, EPS, ALU.mult, ALU.add, [ss2], [ss2])
            act(ss2.t[:, 0:1], ss2.t[:, 0:1], AF.Sqrt, [ss2], [ss2])
            S.op("dve", lambda e: e.reciprocal(rstd2.t[:, 0:1], ss2.t[:, 0:1]), [ss2], [rstd2])
            ts(YN.t[:], YZ.t[:], rstd2.t[:, 0:1], None, ALU.mult, None, [YZ, rstd2], [YN])
            if g == 0:
                tap("yn", j, YN, YN.t[:], 256)
            stage(5)
            yield
            np_ = pnext()
            for i in range(2):
                tr(np_.t[:, i * 128:(i + 1) * 128], YN.t[:, i * 128:(i + 1) * 128], [YN], [np_])
            tt(YNT3[:, 2 * g:2 * g + 2, :].bitcast(F32R), np_.t[:, 0:256].rearrange("p (k t) -> p k t", t=128),
               ngc.t[:, 2 * g:2 * g + 2].unsqueeze(2).to_broadcast([128, 2, 128]), ALU.mult, [np_, ngc], [YNT])
            yield

        yield from rr2([group(g) for g in range(8)])

        V3 = Vb.t[:, 0:1024].rearrange("p (c t) -> p c t", t=128)
        for i in range(4):
            sbp = pnext(); scp = pnext(); sxp = pnext()
            for pb in (sbp, scp, sxp):
                tmq = pnext()
                wbuf, w3 = wget()
                for kt in range(8):
                    mmr(tmq.t[:, 0:256], UT3[:, kt, :], w3[:, kt, :], kt == 0, kt == 7, [UT, wbuf], [tmq])
                wdone()
                TM = tmnext()
                cp(TM.t[:, 0:256], tmq.t[:, 0:256], [tmq], [TM])
                for q in range(2):
                    tr(pb.t[:, q * 128:(q + 1) * 128], TM.t[:, q * 128:(q + 1) * 128], [TM], [pb])
            cp(SCs.t[:], scp.t[:, 0:256], [scp], [SCs])
            for q in range(2):
                c = 2 * i + q
                ph = PH[c]
                tt(ph.t[:, 2:130], sxp.t[:, q * 128:(q + 1) * 128], SCs.t[:, q * 128:(q + 1) * 128], ALU.mult,
                   [sxp, SCs], [ph])
                ts(CACC.t[:], ph.t[:, 0:128], scw3[:, c, 0:1], None, ALU.mult, None, [ph, scw], [CACC])
                for k in range(1, 3):
                    stt(CACC.t[:], ph.t[:, k:k + 128], scw3[:, c, k:k + 1], CACC.t[:], ALU.mult, ALU.add,
                        [ph, scw, CACC], [CACC])
                tt(V3[:, c, :].bitcast(F32R), sbp.t[:, q * 128:(q + 1) * 128], CACC.t[:], ALU.mult, [sbp, CACC], [Vb])
                cp(ph.t[:, 0:2], ph.t[:, 128:130], [ph], [ph], eng="pool")
            yield

        stage(6)
        for cb in range(4):
            cs_ = slice(cb * 256, (cb + 1) * 256)
            for gi in range(2):
                wbuf, w3 = wget()
                gp = pnext()
                for kt in range(8):
                    mmr(gp.t[:, 0:256], UT3[:, kt, :], w3[:, kt, :], kt == 0, kt == 7, [UT, wbuf], [gp])
                wdone()
                act(Gb[gi].t[:, 0:256], gp.t[:, 0:256], AF.Sigmoid, [gp], [Gb[gi]])
            yssd = pnext()
            for kh in range(2):
                wbuf, w3 = wget()
                for kt in range(8):
                    mmr(yssd.t[:, 0:256], YNT3[:, kh * 8 + kt, :], w3[:, kt, :], kh == 0 and kt == 0,
                       kh == 1 and kt == 7, [YNT, wbuf], [yssd])
                wdone()
            tt(MG.t[:, cs_], yssd.t[:, 0:256], Gb[0].t[:, 0:256], ALU.mult, [yssd, Gb[0]], [MG])
            ysc = pnext()
            wbuf, w3 = wget()
            for kt in range(8):
                mmr(ysc.t[:, 0:256], V3[:, kt, :], w3[:, kt, :], kt == 0, kt == 7, [Vb, wbuf], [ysc])
            wdone()
            tt(Gb[1].t[:, 0:256], ysc.t[:, 0:256], Gb[1].t[:, 0:256], ALU.mult, [ysc, Gb[1]], [Gb[1]])
            tt(MG.t[:, cs_], MG.t[:, cs_], Gb[1].t[:, 0:256], ALU.add, [MG, Gb[1]], [MG])
            yield
        transpose8(MG, MGT, r32=True)
        for cb in range(4):
            cs_ = slice(cb * 256, (cb + 1) * 256)
            wbuf, w3 = wget()
            op_ = pnext()
            for kt in range(8):
                mmr(op_.t[:, 0:256], MGT3[:, kt, :], w3[:, kt, :], kt == 0, kt == 7, [MGT, wbuf], [op_])
            wdone()
            tt(H.t[:, cs_], H.t[:, cs_], op_.t[:, 0:256], ALU.add, [H, op_], [H])
            yield
        tap("h2", j, H, H.t[:], 1024)


    def back(j, H, st):
        FF = sm("FF", 1024)
        if with_peer:
            yield from peer_back(nc, S, dict(ENV, j=j, H=H, FF=FF), st)
        else:
            memset(FF.t[:], 0.0, [FF], eng="pool")
        tt(FF.t[:], FF.t[:], H.t[:], ALU.add, [FF, H], [FF])
        ss3 = sm("ss3", 1); rstd3 = sm("rstd3", 1); junk3 = sm("JK", 1024)
        rmsnorm_stats(FF, junk3, ss3, rstd3)
        stt(FF.t[:], FF.t[:], rstd3.t[:, 0:1], LNO, ALU.mult, ALU.mult, [FF, rstd3, rows], [FF])
        S.dma("sp", (lambda e, o=out[(j - 1) * TT:j * TT, :], i=FF.t[:]: e.dma_start(o, i)), FF, reads=[FF],
              is_out=True)
        yield

    def tail(j):
        H = Hr[j % 2]
        st = {}
        if with_peer:
            yield from peer_front(nc, S, dict(ENV, j=j, H=H, FF=sm("FF", 1024)), st)
        yield from back(j, H, st)

    def convert_uv():
        outs = [sm("JK", 1024), sm("FF", 1024)]
        cvs = [Buf("cvs%d" % i, None) for i in range(3)]
        for c in range(128):
            stg = Gfull[c % 3]
            S.dma("sp", (lambda e, o=stg.t[:], i=uv[c * 128:(c + 1) * 128, :]: e.dma_start(o, i)), cvs[c % 3],
                  writes=[stg])
            ob = outs[c % 2]
            cp(ob.t[:].bitcast(BF16), stg.t[:], [stg], [ob], eng="pool")
            S.dma("sp", (lambda e, o=uvb[c * 128:(c + 1) * 128, :], i=ob.t[:].bitcast(BF16): e.dma_start(o, i)), ob,
                  reads=[ob], writes=[UVB])
            yield

    def drain(gen):
        if gen is not None:
            for _ in gen:
                pass

    def interleave(gm, gb, ratio):
        for _ in gm:
            if gb is not None:
                for _k in range(ratio):
                    try:
                        next(gb)
                    except StopIteration:
                        gb = None
                        break
        drain(gb)

    try:
        init_ops()
        pending = convert_uv() if (with_peer and NT > 1) else None
        for j in range(NT):
            interleave(mixer(j), pending, PIPE_RATIO)
            pending = tail(j) if j >= 1 else None
        drain(pending)
    except _Stop:
        pass
    S.replay()
    es.close()
    return nc, es


def peer_front(nc, S, L, st):
    sm = L["sm"]; tt = L["tt"]; ts = L["ts"]; stt = L["stt"]; act = L["act"]; cp = L["cp"]; mm = L["mm"]
    pnext = L["pnext"]; wget = L["wget"]; wdone = L["wdone"]; memset = L["memset"]; tap = L["tap"]; j = L["j"]
    H = L["H"]; rows = L["rows"]; LNF = L["LNF"]; skt = L["skt"]; iota16 = L["iota16"]; uv = L["uv"]
    transpose8 = L["transpose8"]; rmsnorm_stats = L["rmsnorm_stats"]
    Gfull = L["Gfull"]
    QT = Gfull[0]; SCO = Gfull[1]; U2T = sm("U2T", 1024); FF = L["FF"]
    U2 = sm("U2", 1024); ssp = sm("ssp", 1); rsp = sm("rsp", 1)
    rmsnorm_stats(H, FF, ssp, rsp)
    stt(U2.t[:], H.t[:], rsp.t[:, 0:1], LNF, ALU.mult, ALU.mult, [H, rsp, rows], [U2])
    transpose8(U2, U2T)
    U2T3 = U2T.t[:].rearrange("p (k t) -> p k t", t=128)
    QT3 = QT.t[:].rearrange("p (c t) -> p c t", t=128)
    skt3 = skt.t[:].rearrange("p (c k) -> p c k", k=128)
    for i in range(8):
        wbuf, w3 = wget(exact=True)
        qp = pnext()
        for q in range(2):
            for kt in range(8):
                mm(qp.t[:, q * 128:(q + 1) * 128], w3[:, kt, q * 128:(q + 1) * 128], U2T3[:, kt, :],
                   kt == 0, kt == 7, [U2T, wbuf], [qp])
        wdone()
        cp(QT.t[:, i * 256:(i + 1) * 256], qp.t[:, 0:256], [qp], [QT])
        yield
    SCO3 = SCO.t[:].rearrange("p (c k) -> p c k", k=128)
    for b4 in range(4):
        sp_ = pnext()
        for q in range(4):
            c = b4 * 4 + q
            mm(sp_.t[:, q * 128:(q + 1) * 128], QT3[:, c, :], skt3[:, c, :], True, True, [QT, skt], [sp_])
        cp(SCO.t[:, b4 * 512:(b4 + 1) * 512], sp_.t[:, 0:512], [sp_], [SCO])
        yield
    SV = sm("SV", 256); SI = sm("SI", 256, U32); WK = sm("WK", 128); SIF = sm("SIF", 256)
    SV3 = SV.t[:].rearrange("p (c k) -> p c k", k=16)
    SI3 = SI.t[:].rearrange("p (c k) -> p c k", k=16)
    for c in range(16):
        S.op("dve", lambda e, o=SV3[:, c, 0:8], i=SCO3[:, c, :]: e.max(o, i), [SCO], [SV])
        S.op("dve", lambda e, o=WK.t[:], r=SV3[:, c, 0:8], i=SCO3[:, c, :]: e.match_replace(o, r, i, -1.0e30),
             [SCO, SV], [WK])
        S.op("dve", lambda e, o=SV3[:, c, 8:16], i=WK.t[:]: e.max(o, i), [WK], [SV])
        S.op("dve", lambda e, o=SI3[:, c, 0:8], m=SV3[:, c, 0:8], i=SCO3[:, c, :]: e.max_index(o, m, i),
             [SCO, SV], [SI])
        S.op("dve", lambda e, o=SI3[:, c, 8:16], m=SV3[:, c, 8:16], i=SCO3[:, c, :]: e.max_index(o, m, i),
             [SCO, SV], [SI])
        if c % 4 == 3:
            yield
    cp(SIF.t[:], SI.t[:], [SI], [SIF], eng="dve")
    CAND = Gfull[2]; CW = sm("CW", 256); CS = sm("CS", 128); CI = sm("CI", 128, U32); CIF = sm("CIF", 128)
    SV4 = SV.t[:].rearrange("p (h j k) -> p h j k", j=2, k=16)
    SIF4 = SIF.t[:].rearrange("p (h j k) -> p h j k", j=2, k=16)
    CAND3 = CAND.t[:].rearrange("p (h c) -> p h c", c=256)
    CS3 = CS.t[:].rearrange("p (h k) -> p h k", k=16)
    CI3 = CI.t[:].rearrange("p (h k) -> p h k", k=16)
    for h in range(8):
        tt(CAND3[:, h, :].rearrange("p (a b) -> p a b", b=16),
           SV4[:, h, 0, :].unsqueeze(2).to_broadcast([128, 16, 16]),
           SV4[:, h, 1, :].unsqueeze(1).to_broadcast([128, 16, 16]), ALU.add, [SV], [CAND])
    for h in range(8):
        S.op("dve", lambda e, o=CS3[:, h, 0:8], i=CAND3[:, h, :]: e.max(o, i), [CAND], [CS])
        S.op("dve", lambda e, o=CW.t[:], r=CS3[:, h, 0:8], i=CAND3[:, h, :]: e.match_replace(o, r, i, -1.0e30),
             [CAND, CS], [CW])
        S.op("dve", lambda e, o=CS3[:, h, 8:16], i=CW.t[:]: e.max(o, i), [CW], [CS])
        S.op("dve", lambda e, o=CI3[:, h, 0:8], m=CS3[:, h, 0:8], i=CAND3[:, h, :]: e.max_index(o, m, i),
             [CAND, CS], [CI])
        S.op("dve", lambda e, o=CI3[:, h, 8:16], m=CS3[:, h, 8:16], i=CAND3[:, h, :]: e.max_index(o, m, i),
             [CAND, CS], [CI])
        if h % 4 == 3:
            yield
    CA = sm("CA", 128, U32); CB = sm("CB", 128, U32); CAF = sm("CAF", 128); CBF = sm("CBF", 128)
    ts(CA.t[:], CI.t[:], 4, None, ALU.logical_shift_right, None, [CI], [CA])
    ts(CB.t[:], CI.t[:], 15, None, ALU.bitwise_and, None, [CI], [CB])
    cp(CAF.t[:], CA.t[:], [CA], [CAF], eng="dve")
    cp(CBF.t[:], CB.t[:], [CB], [CBF], eng="dve")
    OH = sm("OH", 1024); I1 = sm("I1", 128); I2 = sm("I2", 128); EIF = sm("EIF", 128); EI = sm("EI", 128, U32)
    for (sel, jj, dst) in ((CAF, 0, I1), (CBF, 1, I2)):
        for half in range(2):
            hs = slice(half * 4, half * 4 + 4)
            OH4 = OH.t[:].rearrange("p (h k a) -> p h k a", k=16, a=16)
            selv = sel.t[:].rearrange("p (h k) -> p h k", k=16)[:, hs, :]
            for hh in range(4):
                h = half * 4 + hh
                tt(OH4[:, hh, :, :], sel.t[:].rearrange("p (h k) -> p h k", k=16)[:, h, :].unsqueeze(2).to_broadcast([128, 16, 16]),
                   iota16.t[:].unsqueeze(1).to_broadcast([128, 16, 16]), ALU.is_equal, [sel, iota16], [OH])
                tt(OH4[:, hh, :, :], OH4[:, hh, :, :],
                   SIF4[:, h, jj, :].unsqueeze(1).to_broadcast([128, 16, 16]), ALU.mult, [OH, SIF], [OH])
            S.op("dve", lambda e, o=dst.t[:, half * 64:(half + 1) * 64], i=OH.t[:].rearrange("p (x a) -> p x a", a=16):
                 e.tensor_reduce(o, i, AX.X, ALU.add), [OH], [dst])
    stt(EIF.t[:], I1.t[:], 128.0, I2.t[:], ALU.mult, ALU.add, [I1, I2], [EIF])
    ts(EIF.t[:], EIF.t[:], 0.0, 16383.0, ALU.max, ALU.min, [EIF], [EIF])
    cp(EI.t[:], EIF.t[:], [EIF], [EI], eng="dve")
    tap("eif", j, EIF, EIF.t[:], 128)
    GW = sm("GW", 128); GS = sm("GS", 8); GR = sm("GR", 8)
    GW3 = GW.t[:].rearrange("p (h k) -> p h k", k=16)
    tt(GW3, CS3, CS3[:, :, 0:1].to_broadcast([128, 8, 16]), ALU.subtract, [CS], [GW])
    act(GW.t[:], GW.t[:], AF.Exp, [GW], [GW])
    S.op("dve", lambda e: e.tensor_reduce(GS.t[:], GW3, AX.X, ALU.add), [GW], [GS])
    S.op("dve", lambda e: e.reciprocal(GR.t[:], GS.t[:]), [GS], [GR])
    tt(GW3, GW3, GR.t[:].unsqueeze(2).to_broadcast([128, 8, 16]), ALU.mult, [GW, GR], [GW])
    tap("gw", j, GW, GW.t[:], 128)
    st.update(EI=EI, GW=GW, U2=U2)
    yield


def peer_back(nc, S, L, st):
    sm = L["sm"]; tt = L["tt"]; stt = L["stt"]; act = L["act"]; memset = L["memset"]; tap = L["tap"]; j = L["j"]
    uv = L["uv"]; FF = L["FF"]
    EI = st["EI"]; GW = st["GW"]; U2 = st["U2"]
    Gh = L["Gh"]; gslot = L["gslot"]; uvb = L["uvb"]; UVB = L["UVB"]
    NS = 6
    ts = L["ts"]; mm = L["mm"]; cp = L["cp"]; FFP = L["FFP"]; identb = L["identb"]; DGr = L["DGr"]
    AP_ = sm("APRE", 128); GEL = sm("GEL", 128); GA = sm("GA", 128); JK = sm("JK", 1024)

    def gate(s):
        dg = DGr[s % 3]
        ts(dg.t[:], identb.t[:], GEL.t[:, s:s + 1], GW.t[:, s:s + 1], ALU.mult, ALU.mult, [identb, GEL, GW], [dg])

    def accum(s):
        k = s % NS
        dg = DGr[s % 3]
        for half in range(2):
            mm(FFP[half].t[:, 0:512], dg.t[:], gslot(k)[:, 1024 + 512 * half:1024 + 512 * (half + 1)],
               s == 0, s == 127, [dg, Gh[k]], [FFP[half]])

    for s in range(128):
        k = s % NS
        gb = Gh[k]
        S.dma("pool", (lambda e, o=gslot(k), idx=EI.t[:, s:s + 1]: e.indirect_dma_start(
            o, None, uvb, bass.IndirectOffsetOnAxis(idx, 0))), gb, reads=[EI, UVB], writes=[gb])
        S.op("dve", lambda e, o=JK.t[:], a=gslot(k)[:, 0:1024], acc=AP_.t[:, s:s + 1]: e.scalar_tensor_tensor(
            o, a, 1.0, U2.t[:], ALU.mult, ALU.mult, accum_out=acc), [gb, U2], [JK, AP_], indep=True)
        act(GEL.t[:, s:s + 1], AP_.t[:, s:s + 1], AF.Gelu, [AP_], [GEL], indep=True)
        if s >= 2:
            gate(s - 2)
        if s >= 3:
            accum(s - 3)
        yield
    gate(126); accum(125)
    gate(127); accum(126); accum(127)
    for half in range(2):
        cp(FF.t[:, 512 * half:512 * (half + 1)], FFP[half].t[:, 0:512], [FFP[half]], [FF], eng="dve")
    tap("ff", j, FF, FF.t[:], 1024)


def _blk(W):
    return np.ascontiguousarray(W.reshape(8, 128, 256).transpose(1, 0, 2)).reshape(128, 2048)


def prep_shared(inp):
    f = np.float32
    w_in = np.asarray(inp["w_in"], f)[0]
    blocks = []
    for g in range(8):
        blocks.append(w_in[:, 256 * g:256 * g + 256])
        blocks.append(w_in[:, 2048 + 256 * g:2048 + 256 * g + 256])
        blocks.append(np.concatenate([w_in[:, 4096 + 128 * g:4096 + 128 * g + 128],
                                      w_in[:, 5120 + 128 * g:5120 + 128 * g + 128]], 1))
    o = 6176
    for i in range(4):
        for k in range(3):
            blocks.append(w_in[:, o + 1024 * k + 256 * i:o + 1024 * k + 256 * i + 256])
    og = 9248
    wout = np.asarray(inp["ssd_w_out"], f)[0]
    scw_out = np.asarray(inp["sc_w_out"], f)[0]
    for cb in range(4):
        c0, c1 = 256 * cb, 256 * cb + 256
        blocks.append(w_in[:, og + c0:og + c1])
        blocks.append(w_in[:, og + 1024 + c0:og + 1024 + c1])
        blocks.append(wout[0:1024, c0:c1])
        blocks.append(wout[1024:2048, c0:c1])
        blocks.append(scw_out[:, c0:c1])
    w_o = np.asarray(inp["w_o"], f)[0]
    for cb in range(4):
        blocks.append(w_o[:, 256 * cb:256 * cb + 256])
    wq = np.asarray(inp["peer_w_q"], f)[0]
    for i in range(8):
        blocks.append(wq[:, 256 * i:256 * i + 256])
    assert len(blocks) == NBLK
    wall = np.stack([_blk(b) for b in blocks]).astype(f)
    sh = {"wall": wall}
    sh["uv"] = np.ascontiguousarray(np.concatenate([np.asarray(inp["peer_u"], f)[0],
                                                    np.asarray(inp["peer_v"], f)[0]], axis=1))
    sh["c_ident"] = np.eye(128, dtype=f)
    tri = np.triu(np.ones((128, 128), f))
    sh["c_tri"] = tri
    sh["c_negm"] = ((tri - 1.0) * (-NEG)).astype(f)
    sh["c_ones"] = np.ones((128, 128), f)
    sh["c_iota"] = np.tile(np.arange(16, dtype=f)[None, :], (128, 1))
    row = np.concatenate([np.asarray(inp["ln_ffn"], f)[0], np.asarray(inp["ln_final"], f),
                          np.asarray(inp["ssd_dt_bias"], f)[0], np.asarray(inp["ssd_a_log"], f)[0],
                          np.asarray(inp["ssd_d"], f)[0]])
    sh["c_rows"] = np.ascontiguousarray(np.tile(row[None, :], (128, 1)))
    cwf = np.asarray(inp["ssd_conv_w"], f)[0]
    cbf = np.asarray(inp["ssd_conv_b"], f)[0]
    cw = np.zeros((128, 32, 5), f)
    for g in range(8):
        for i in range(4):
            if i < 2:
                ch = 256 * g + 128 * i
            elif i == 2:
                ch = 2048 + 128 * g
            else:
                ch = 3072 + 128 * g
            cw[:, 4 * g + i, 0:4] = cwf[ch:ch + 128, :]
            cw[:, 4 * g + i, 4] = cbf[ch:ch + 128]
    sh["c_cw"] = cw.reshape(128, 160)
    sh["c_scw"] = np.ascontiguousarray(np.asarray(inp["sc_conv_w"], f)[0].reshape(8, 128, 3).transpose(1, 0, 2)).reshape(128, 24)
    sh["c_wdt"] = np.ascontiguousarray(w_in[:, 6144:6176].reshape(8, 128, 32).transpose(1, 0, 2)).reshape(128, 256)
    sk = np.asarray(inp["peer_sub_keys"], f)[0].reshape(16, 128, 128)
    sh["c_skt"] = np.ascontiguousarray(sk.transpose(2, 0, 1)).reshape(128, 2048)
    sh["c_lnmix"] = np.ascontiguousarray(np.asarray(inp["ln_mix"], f)[0].reshape(8, 128).T)
    sh["c_ngc"] = np.ascontiguousarray(np.asarray(inp["ssd_norm"], f)[0].reshape(16, 128).T)
    return sh


def make_xh(inp, b):
    f = np.float32
    xh = np.zeros((NTOK_EXT, D), f)
    xh[112:128] = np.asarray(inp["meta_tokens"], f)
    xh[128:] = np.asarray(inp["x"], f)[b]
    return xh


_CACHE = {}


def kernel(**inputs):
    if "nc" not in _CACHE:
        _CACHE["nc"] = build()
    nc, _es = _CACHE["nc"]
    sh = prep_shared(inputs)
    in_maps = []
    for b in range(8):
        m = dict(sh)
        m["xh"] = make_xh(inputs, b)
        in_maps.append(m)
    res = run_bass_kernel_spmd(nc, in_maps, core_ids=list(range(8)))
    return np.stack([np.asarray(r["out"], np.float32) for r in res.results], axis=0)
```
